# Optimizing a Trainium2 kernel written in Bass

```python
import numpy as np
import jax
import jax.numpy as jnp
from jax import lax

D_MODEL = 2048
BATCH = 4
SEQ = 2048
DEPTH = 2

H_A = 8
KV_A = 2
HPG_A = H_A // KV_A
HD_A = 128
L_CMP = 32
STRIDE_CMP = 16
L_SEL = 64
N_SEL = 8
WIN_A = 512
H_B = 8
KV_B = 2
HPG_B = H_B // KV_B
HD_B = 64
WIN_B = 128
H_C = 4
HD_C = 128
BLK_C = 256
TOPK_C = 3
Q_BLOCK = 128
GATHER_CHUNK = 64
N_GROUPS = 4
EXP_PER_GROUP = 8
N_EXPERTS = N_GROUPS * EXP_PER_GROUP
D_EXPERT = 256
TOPK_EXPERT = 2

LN_EPS = 1e-5
NEG_INF = -1e30

Q_A_W = H_A * HD_A
KV_A_W = 3 * 2 * KV_A * HD_A
GATE_A_W = 3 * H_A
Q_B_W = H_B * HD_B
KV_B_W = KV_B * HD_B
C_W = H_C * HD_C
MERGE_W = 3 * D_MODEL
IN_WIDTHS = (Q_A_W, KV_A_W, GATE_A_W, Q_B_W, KV_B_W, KV_B_W, C_W, C_W, C_W, MERGE_W)
D_IN = sum(IN_WIDTHS)

kernel_name = 'hybrid_nsa_swa_moba_hmoe'


def alibi_slopes(n):
    return jnp.asarray([2.0 ** (-8.0 * (i + 1) / n) for i in range(n)], jnp.float32)


def layer_norm(x, g, b):
    xf = x.astype(jnp.float32)
    mu = jnp.mean(xf, -1, keepdims=True)
    xc = xf - mu
    var = jnp.mean(xc * xc, -1, keepdims=True)
    return xc * lax.rsqrt(var + LN_EPS) * g.astype(jnp.float32) + b.astype(jnp.float32)


def banded_window_attention(q, k, v, slopes, window, sink=None):
    B, G, H, S, d = q.shape
    nqb = S // Q_BLOCK
    span = window + Q_BLOCK
    pad = ((0, 0), (0, 0), (window, 0), (0, 0))
    kidx = Q_BLOCK * np.arange(nqb)[:, None] + np.arange(span)[None, :]
    kw = jnp.pad(k, pad)[:, :, kidx]
    vw = jnp.pad(v, pad)[:, :, kidx]
    qb = q.reshape(B, G, H, nqb, Q_BLOCK, d)
    t_pos = Q_BLOCK * np.arange(nqb)[:, None] + np.arange(Q_BLOCK)[None, :]
    s_pos = kidx - window
    dist = t_pos[:, :, None] - s_pos[:, None, :]
    mask = jnp.asarray((dist >= 0) & (dist < window) & (s_pos[:, None, :] >= 0))
    s = jnp.einsum('bghcqd,bgckd->bghcqk', qb, kw, preferred_element_type=jnp.float32) * (d ** -0.5)
    s = s - slopes[:, :, None, None, None] * jnp.asarray(dist, jnp.float32)
    s = jnp.where(mask, s, NEG_INF)
    if sink is not None:
        sink_col = jnp.broadcast_to(sink.astype(jnp.float32)[:, :, None, None, None], s.shape[:-1] + (1,))
        p = jax.nn.softmax(jnp.concatenate([s, sink_col], -1), -1)[..., :-1]
    else:
        p = jax.nn.softmax(s, -1)
    o = jnp.einsum('bghcqk,bgckd->bghcqd', p, vw)
    return o.reshape(B, G, H, S, d)


def gathered_block_attention(q, k_blocks, v_blocks, blk_idx, blk_ok, slopes, with_own_block):
    B, G, H, S, d = q.shape
    L = k_blocks.shape[3]
    n = blk_idx.shape[-1]
    nch = S // GATHER_CHUNK
    scale = d ** -0.5
    offs = jnp.arange(L)
    sl = slopes.astype(jnp.float32)
    take = jax.vmap(jax.vmap(lambda blocks, idx: blocks[idx]))
    q_ch = jnp.moveaxis(q.reshape(B, G, H, nch, GATHER_CHUNK, d), 3, 0)
    i_ch = jnp.moveaxis(blk_idx.reshape(B, G, nch, GATHER_CHUNK, n), 2, 0)
    ok_ch = jnp.moveaxis(blk_ok.reshape(B, G, nch, GATHER_CHUNK, n), 2, 0)

    def chunk(args):
        q_c, idx_c, ok_c, c = args
        t = c * GATHER_CHUNK + jnp.arange(GATHER_CHUNK)
        kg = take(k_blocks, idx_c)
        vg = take(v_blocks, idx_c)
        dist = t[:, None, None] - (idx_c[..., None] * L + offs)
        mask = ok_c[..., None] & (dist >= 0)
        s = jnp.einsum('bghqd,bgqnld->bghqnl', q_c, kg, preferred_element_type=jnp.float32) * scale
        s = s - sl[:, :, None, None, None] * dist[:, :, None].astype(jnp.float32)
        s = jnp.where(mask[:, :, None], s, NEG_INF).reshape(B, G, H, GATHER_CHUNK, n * L)
        parts = [s]
        if with_own_block:
            own = (c * GATHER_CHUNK) // L
            k_own = lax.dynamic_index_in_dim(k_blocks, own, axis=2, keepdims=False)
            v_own = lax.dynamic_index_in_dim(v_blocks, own, axis=2, keepdims=False)
            dist_o = t[:, None] - (own * L + offs)[None, :]
            s_o = jnp.einsum('bghqd,bgld->bghql', q_c, k_own, preferred_element_type=jnp.float32) * scale
            s_o = jnp.where(dist_o >= 0, s_o - sl[:, :, None, None] * dist_o.astype(jnp.float32), NEG_INF)
            parts.append(s_o)
        p = jax.nn.softmax(jnp.concatenate(parts, -1), -1)
        o = jnp.einsum('bghqnl,bgqnld->bghqd', p[..., :n * L].reshape(B, G, H, GATHER_CHUNK, n, L), vg)
        if with_own_block:
            o = o + jnp.einsum('bghql,bgld->bghqd', p[..., n * L:], v_own)
        return o

    out = lax.map(chunk, (q_ch, i_ch, ok_ch, jnp.arange(nch)))
    return jnp.moveaxis(out, 0, 3).reshape(B, G, H, S, d)


def nsa_mixer(q, kv, gate_logits, cmp_pos, cmp_w):
    B, S, _ = q.shape
    f32 = jnp.float32
    q = q.reshape(B, S, KV_A, HPG_A, HD_A).transpose(0, 2, 3, 1, 4)
    kv = kv.reshape(B, S, 3, 2, KV_A, HD_A).transpose(2, 3, 0, 4, 1, 5)
    slopes = alibi_slopes(H_A).reshape(KV_A, HPG_A)
    pos = np.arange(S)

    n_cmp = (S - L_CMP) // STRIDE_CMP + 1
    c_start = STRIDE_CMP * np.arange(n_cmp)
    c_idx = c_start[:, None] + np.arange(L_CMP)[None, :]
    k_cmp = jnp.einsum('bgnld,lde->bgne', kv[0, 0][:, :, c_idx] + cmp_pos[0], cmp_w[0])
    v_cmp = jnp.einsum('bgnld,lde->bgne', kv[0, 1][:, :, c_idx] + cmp_pos[1], cmp_w[1])
    dist = pos[:, None] - (c_start + L_CMP - 1)[None, :]
    ok = jnp.asarray(dist >= 0)
    s = jnp.einsum('bghtd,bgnd->bghtn', q, k_cmp, preferred_element_type=f32) * (HD_A ** -0.5)
    s = s - slopes[:, :, None, None] * jnp.asarray(dist, f32)
    p_cmp = jax.nn.softmax(jnp.where(ok, s, NEG_INF), -1) * ok
    o_cmp = jnp.einsum('bghtn,bgnd->bghtd', p_cmp, v_cmp)

    n_sel = S // L_SEL
    s_start = L_SEL * np.arange(n_sel)
    inter = np.clip(np.minimum(c_start[:, None] + L_CMP, s_start[None, :] + L_SEL)
                    - np.maximum(c_start[:, None], s_start[None, :]), 0, None) / L_CMP
    imp = jnp.einsum('bghtn,nj->bgtj', p_cmp, jnp.asarray(inter, f32))
    blk_t = pos // L_SEL
    j = np.arange(n_sel)
    valid = j[None, :] <= blk_t[:, None]
    forced = (j[None, :] == 0) | (j[None, :] == blk_t[:, None]) | (j[None, :] == blk_t[:, None] - 1)
    imp = jnp.where(jnp.asarray(forced), jnp.inf, jnp.where(jnp.asarray(valid), imp, -jnp.inf))
    _, sel = lax.top_k(imp, min(N_SEL, n_sel))
    sel_ok = sel <= jnp.asarray(blk_t)[:, None]
    kb = kv[1, 0].reshape(B, KV_A, n_sel, L_SEL, HD_A)
    vb = kv[1, 1].reshape(B, KV_A, n_sel, L_SEL, HD_A)
    o_slc = gathered_block_attention(q, kb, vb, sel, sel_ok, slopes, False)

    o_win = banded_window_attention(q, kv[2, 0], kv[2, 1], slopes, WIN_A)

    g = jax.nn.sigmoid(gate_logits.astype(f32)).reshape(B, S, KV_A, HPG_A, 3).transpose(0, 2, 3, 1, 4)[..., None]
    o = g[..., 0, :] * o_cmp + g[..., 1, :] * o_slc + g[..., 2, :] * o_win
    return o.transpose(0, 3, 1, 2, 4).reshape(B, S, Q_A_W)


def swa_sink_mixer(q, k, v, sink):
    B, S, _ = q.shape
    q = q.reshape(B, S, KV_B, HPG_B, HD_B).transpose(0, 2, 3, 1, 4)
    k = k.reshape(B, S, KV_B, HD_B).transpose(0, 2, 1, 3)
    v = v.reshape(B, S, KV_B, HD_B).transpose(0, 2, 1, 3)
    slopes = alibi_slopes(H_B).reshape(KV_B, HPG_B)
    o = banded_window_attention(q, k, v, slopes, WIN_B, sink.reshape(KV_B, HPG_B))
    return o.transpose(0, 3, 1, 2, 4).reshape(B, S, Q_B_W)


def moba_mixer(q, k, v):
    B, S, _ = q.shape
    f32 = jnp.float32
    s_pad = -(-S // BLK_C) * BLK_C
    nb = s_pad // BLK_C
    padw = ((0, 0), (0, 0), (0, s_pad - S), (0, 0))
    q = jnp.pad(q.reshape(B, S, H_C, HD_C).transpose(0, 2, 1, 3), padw)
    k = jnp.pad(k.reshape(B, S, H_C, HD_C).transpose(0, 2, 1, 3), padw)
    v = jnp.pad(v.reshape(B, S, H_C, HD_C).transpose(0, 2, 1, 3), padw)
    kb = k.reshape(B, H_C, nb, BLK_C, HD_C)
    vb = v.reshape(B, H_C, nb, BLK_C, HD_C)
    k_mean = jnp.mean(kb.astype(f32), axis=3)
    score = jnp.einsum('bhtd,bhnd->bhtn', q, k_mean, preferred_element_type=f32)
    blk_t = np.arange(s_pad) // BLK_C
    past = jnp.asarray(np.arange(nb)[None, :] < blk_t[:, None])
    _, sel = lax.top_k(jnp.where(past, score, -jnp.inf), min(TOPK_C, nb))
    sel_ok = sel < jnp.asarray(blk_t)[:, None]
    o = gathered_block_attention(q[:, :, None], kb, vb, sel, sel_ok, alibi_slopes(H_C).reshape(H_C, 1), True)
    return o[:, :, 0, :S].transpose(0, 2, 1, 3).reshape(B, S, C_W)


def mixer_sublayer(x, w_in, cmp_pos, cmp_w, sink, w_br_a, w_br_b, w_br_c, w_out):
    B, S, D = x.shape
    z = jnp.einsum('bsd,dc->bsc', x, w_in)
    offsets = np.cumsum(IN_WIDTHS)[:-1].tolist()
    q_a, kv_a, gate_a, q_b, k_b, v_b, q_c, k_c, v_c, gate_m = jnp.split(z, offsets, axis=-1)
    o_a = nsa_mixer(q_a, kv_a, gate_a, cmp_pos, cmp_w)
    o_b = swa_sink_mixer(q_b, k_b, v_b, sink)
    o_c = moba_mixer(q_c, k_c, v_c)
    gm = jax.nn.sigmoid(gate_m.astype(jnp.float32)).reshape(B, S, 3, D)
    merged = gm[:, :, 0] * (o_a @ w_br_a) + gm[:, :, 1] * (o_b @ w_br_b) + gm[:, :, 2] * (o_c @ w_br_c)
    return merged.astype(x.dtype) @ w_out


def hierarchical_moe(h, w_group, b_group, w_router, b_router, w_gate, w_up, w_down):
    B, S, D = h.shape
    f32 = jnp.float32
    t = h.reshape(B * S, D)
    g_prob = jax.nn.softmax((t @ w_group).astype(f32) + b_group.astype(f32), -1)
    g_w, g_idx = lax.top_k(g_prob, 1)
    e_logits = ((t @ w_router).astype(f32) + b_router.astype(f32)).reshape(-1, N_GROUPS, EXP_PER_GROUP)
    e_in = jnp.take_along_axis(e_logits, g_idx[:, :, None], axis=1)[:, 0]
    e_top, e_idx = lax.top_k(e_in, TOPK_EXPERT)
    w = jax.nn.softmax(e_top, -1) * g_w
    combine = jnp.einsum('tk,tke->te', w, jax.nn.one_hot(g_idx * EXP_PER_GROUP + e_idx, N_EXPERTS, dtype=f32))
    a = jax.nn.silu(jnp.einsum('td,edf->tef', t, w_gate)) * jnp.einsum('td,edf->tef', t, w_up)
    y = jnp.einsum('tef,efd->td', a * combine[:, :, None].astype(a.dtype), w_down)
    return y.reshape(B, S, D).astype(h.dtype)


def setup_inputs(seed: int = 0) -> dict:
    key = jax.random.key(seed)
    ks = jax.random.split(key, 32)
    f32 = jnp.float32
    beta = (8.0 * DEPTH) ** -0.25
    s_d = D_MODEL ** -0.5

    def nrm(k, shape, scale):
        return jax.random.normal(k, shape, f32) * scale

    kv_a = nrm(ks[2], (DEPTH, D_MODEL, 3, 2, KV_A * HD_A), s_d) * jnp.asarray([1.0, beta], f32)[:, None]
    w_in = jnp.concatenate([
        nrm(ks[1], (DEPTH, D_MODEL, Q_A_W), s_d),
        kv_a.reshape(DEPTH, D_MODEL, KV_A_W),
        nrm(ks[3], (DEPTH, D_MODEL, GATE_A_W), s_d),
        nrm(ks[4], (DEPTH, D_MODEL, Q_B_W), s_d),
        nrm(ks[5], (DEPTH, D_MODEL, KV_B_W), s_d),
        nrm(ks[6], (DEPTH, D_MODEL, KV_B_W), s_d * beta),
        nrm(ks[7], (DEPTH, D_MODEL, C_W), s_d),
        nrm(ks[8], (DEPTH, D_MODEL, C_W), s_d),
        nrm(ks[9], (DEPTH, D_MODEL, C_W), s_d * beta),
        nrm(ks[10], (DEPTH, D_MODEL, MERGE_W), s_d)], axis=-1)
    return {
        'x': jax.random.normal(ks[0], (BATCH, SEQ, D_MODEL), f32),
        'w_in': w_in,
        'nsa_cmp_pos': nrm(ks[11], (DEPTH, 2, L_CMP, HD_A), 0.1),
        'nsa_cmp_w': nrm(ks[12], (DEPTH, 2, L_CMP, HD_A, HD_A), (L_CMP * HD_A) ** -0.5),
        'sink_b': nrm(ks[13], (DEPTH, H_B), 0.5),
        'w_br_a': nrm(ks[14], (DEPTH, Q_A_W, D_MODEL), Q_A_W ** -0.5),
        'w_br_b': nrm(ks[15], (DEPTH, Q_B_W, D_MODEL), Q_B_W ** -0.5),
        'w_br_c': nrm(ks[16], (DEPTH, C_W, D_MODEL), C_W ** -0.5),
        'w_out': nrm(ks[17], (DEPTH, D_MODEL, D_MODEL), s_d * beta),
        'ln1_g': 1.0 + nrm(ks[18], (DEPTH, D_MODEL), 0.02),
        'ln1_b': nrm(ks[19], (DEPTH, D_MODEL), 0.02),
        'w_group': nrm(ks[20], (DEPTH, D_MODEL, N_GROUPS), s_d),
        'b_group': nrm(ks[21], (DEPTH, N_GROUPS), 0.01),
        'w_router': nrm(ks[22], (DEPTH, D_MODEL, N_EXPERTS), s_d),
        'b_router': nrm(ks[23], (DEPTH, N_EXPERTS), 0.01),
        'w_gate': nrm(ks[24], (DEPTH, N_EXPERTS, D_MODEL, D_EXPERT), s_d),
        'w_up': nrm(ks[25], (DEPTH, N_EXPERTS, D_MODEL, D_EXPERT), s_d),
        'w_down': nrm(ks[26], (DEPTH, N_EXPERTS, D_EXPERT, D_MODEL), D_EXPERT ** -0.5 * beta),
        'ln2_g': 1.0 + nrm(ks[27], (DEPTH, D_MODEL), 0.02),
        'ln2_b': nrm(ks[28], (DEPTH, D_MODEL), 0.02),
    }


def reference(x, w_in, nsa_cmp_pos, nsa_cmp_w, sink_b, w_br_a, w_br_b, w_br_c, w_out,
              ln1_g, ln1_b, w_group, b_group, w_router, b_router, w_gate, w_up, w_down,
              ln2_g, ln2_b):
    alpha = (2.0 * DEPTH) ** 0.25
    for l in range(DEPTH):
        y = mixer_sublayer(x, w_in[l], nsa_cmp_pos[l], nsa_cmp_w[l], sink_b[l],
                           w_br_a[l], w_br_b[l], w_br_c[l], w_out[l])
        x = layer_norm(alpha * x + y, ln1_g[l], ln1_b[l]).astype(x.dtype)
        y = hierarchical_moe(x, w_group[l], b_group[l], w_router[l], b_router[l],
                             w_gate[l], w_up[l], w_down[l])
        x = layer_norm(alpha * x + y, ln2_g[l], ln2_b[l]).astype(x.dtype)
    return x
```

```python
import numpy as np
import concourse.bass as bass
import concourse.mybir as mybir
from concourse.bass_utils import run_bass_kernel_spmd

F32 = mybir.dt.float32
BF16 = mybir.dt.bfloat16
AF = mybir.ActivationFunctionType
ALU = mybir.AluOpType
AX = mybir.AxisListType

SEM_LIMIT = 3000


class Buf:
    def __init__(self, name, t=None):
        self.name = name
        self.t = t
        self.last_write = None
        self.reads = []

    def ap(self):
        return self.t[:] if not hasattr(self.t, "ap") else self.t.ap()

    def __getitem__(self, idx):
        return self.t[idx]


class _Eng:
    def __init__(self, name, handle):
        self.name = name
        self.h = handle
        self.sem = None
        self.count = 0
        self.seen = {}


class Sched:
    def __init__(self, nc, n_dma_sems=32):
        self.nc = nc
        self.engs = {
            "pe": _Eng("pe", nc.tensor),
            "act": _Eng("act", nc.scalar),
            "dve": _Eng("dve", nc.vector),
            "pool": _Eng("pool", nc.gpsimd),
            "sp": _Eng("sp", nc.sync),
        }
        self.sems = {}
        self._nsem = 0
        for e in self.engs.values():
            self._new_eng_sem(e)
        self.dma_sems = []
        for i in range(n_dma_sems):
            k = self._alloc_sem("dma%d" % i)
            self.dma_sems.append([k, 0])
        self.dma_rr = 0
        self.ninst = 0

    def _alloc_sem(self, name):
        self._nsem += 1
        key = "%s_%d" % (name, self._nsem)
        self.sems[key] = self.nc.alloc_semaphore(name=key)
        return key

    def _new_eng_sem(self, e):
        e.sem = self._alloc_sem("c_" + e.name)
        e.count = 0

    def sb(self, name, shape, dtype):
        return Buf(name, self.nc.alloc_sbuf_tensor(name, list(shape), dtype))

    def ps(self, name, shape, dtype=F32):
        return Buf(name, self.nc.alloc_psum_tensor(name, list(shape), dtype))

    def tok(self, name):
        return Buf(name, None)

    def _deps(self, reads, writes):
        need = {}
        for b in reads:
            if b.last_write is not None:
                k, v = b.last_write
                need[k] = max(need.get(k, 0), v)
        for b in writes:
            if b.last_write is not None:
                k, v = b.last_write
                need[k] = max(need.get(k, 0), v)
            for k, v in b.reads:
                need[k] = max(need.get(k, 0), v)
        return need

    def _emit_waits(self, e, need):
        for k, v in need.items():
            if e.seen.get(k, 0) < v:
                e.h.wait_ge(self.sems[k], v)
                e.seen[k] = v

    def _record(self, reads, writes, key, val):
        for b in reads:
            b.reads.append((key, val))
        for b in writes:
            b.last_write = (key, val)
            b.reads = []

    def op(self, eng, fn, reads=(), writes=()):
        e = self.engs[eng]
        if e.count >= SEM_LIMIT:
            self._new_eng_sem(e)
        need = self._deps(reads, writes)
        self._emit_waits(e, need)
        inst = fn()
        e.count += 1
        inst.then_inc(self.sems[e.sem], 1)
        if eng == "pe":
            e.seen[e.sem] = e.count
        self._record(reads, writes, e.sem, e.count)
        self.ninst += 1
        return inst

    def dma(self, queue, out, in_, reads=(), writes=(), **kw):
        e = self.engs[queue]
        slot = self.dma_sems[self.dma_rr]
        self.dma_rr = (self.dma_rr + 1) % len(self.dma_sems)
        key, cur = slot
        if cur + 16 > SEM_LIMIT:
            key = self._alloc_sem("dmax")
            slot[0] = key
            cur = 0
        need = self._deps(reads, writes)
        if cur > 0:
            need[key] = max(need.get(key, 0), cur)
        self._emit_waits(e, need)
        inst = e.h.dma_start(out=out, in_=in_, **kw)
        inst.then_inc(self.sems[key], 16)
        slot[1] = cur + 16
        self._record(reads, writes, key, cur + 16)
        self.ninst += 1
        return inst

    def finish(self):
        e = self.engs["sp"]
        need = {}
        for key, cur in self.dma_sems:
            if cur > 0:
                need[key] = cur
        for o in self.engs.values():
            if o is not e and o.count > 0:
                need[o.sem] = o.count
        self._emit_waits(e, need)


D = 2048
KC = 16
TOK = 2048
NT = TOK // 128
DEPTH = 2
ALPHA = (2.0 * DEPTH) ** 0.25
LN_EPS = 1e-5
D_IN = 11032
OFF_QA, OFF_KVA, OFF_GA, OFF_QB, OFF_KB, OFF_VB, OFF_QC, OFF_KC_, OFF_VC, OFF_MG = (
    0, 1024, 2560, 2584, 3096, 3224, 3352, 3864, 4376, 4888)
BIG = 1.0e30
NEGB = 30000.0
SC_A = 128.0 ** -0.5
SC_B = 64.0 ** -0.5


class Ring:
    def __init__(self, bufs):
        self.bufs = bufs
        self.i = 0

    def next(self):
        b = self.bufs[self.i % len(self.bufs)]
        self.i += 1
        return b


class Phase:
    def __init__(self, S):
        self.S = S

    def __enter__(self):
        from contextlib import ExitStack
        self.stack = ExitStack()
        self.prev = getattr(self.S, "stack", None)
        self.S.stack = self.stack
        return self

    def __exit__(self, *a):
        self.S.barrier()
        self.stack.close()
        self.S.stack = self.prev
        return False


def _phase(self):
    return Phase(self)


def _sbp(self, name, shape, dtype):
    self._nbuf = getattr(self, "_nbuf", 0) + 1
    nm = "%s_%d" % (name, self._nbuf)
    t = self.stack.enter_context(self.nc.sbuf_tensor(nm, list(shape), dtype))
    return Buf(nm, t)


def _barrier(self):
    engs = list(self.engs.values())
    need_all = {}
    for key, cur in self.dma_sems:
        if cur > 0:
            need_all[key] = cur
    for o in engs:
        if o.count > 0:
            need_all[o.sem] = o.count
    for e in engs:
        self._emit_waits(e, dict(need_all))


def _mm(self, out, lhsT, rhs, start=True, stop=True, reads=(), writes=()):
    nc = self.nc
    return self.op("pe", lambda: nc.tensor.matmul(out, lhsT, rhs, start=start, stop=stop),
                   reads=reads, writes=writes)


def _tr(self, out, in_, ident, reads=(), writes=()):
    nc = self.nc
    return self.op("pe", lambda: nc.tensor.transpose(out, in_, ident), reads=reads, writes=writes)


def _copy(self, eng, out, in_, reads=(), writes=()):
    nc = self.nc
    if eng == "act":
        f = lambda: nc.scalar.copy(out, in_)
    elif eng == "dve":
        f = lambda: nc.vector.tensor_copy(out, in_)
    else:
        f = lambda: nc.gpsimd.tensor_copy(out, in_)
    return self.op(eng, f, reads=reads, writes=writes)


Sched.phase = _phase
Sched.sbp = _sbp
Sched.barrier = _barrier
Sched.mm = _mm
Sched.tr = _tr
Sched.copy = _copy


class Ctx:
    pass


def build_program(n_layers=DEPTH, debug=None):
    nc = bass.Bass("TRN2", target_bir_lowering=False)
    S = Sched(nc)
    C = Ctx()
    C.nc, C.S = nc, S
    dbg = debug or ()
    C.dbg = dbg

    def din(name, shape, dt=F32):
        return nc.dram_tensor(name, list(shape), dt, kind="ExternalInput").ap()

    def dscr(name, shape, dt):
        if name in dbg:
            return nc.dram_tensor(name, list(shape), dt, kind="ExternalOutput").ap()
        return nc.dram_tensor(name, list(shape), dt).ap()

    shapes = {
        "x": [TOK, D], "w_in": [DEPTH, D, D_IN], "nsa_cmp_pos": [DEPTH, 64, 128],
        "nsa_cmp_w": [DEPTH, 2, 32, 128, 128], "sink_b": [DEPTH, 8], "w_br_a": [DEPTH, 1024, D],
        "w_br_b": [DEPTH, 512, D], "w_br_c": [DEPTH, 512, D], "w_out": [DEPTH, D, D],
        "ln1_g": [DEPTH, D], "ln1_b": [DEPTH, D], "ln2_g": [DEPTH, D], "ln2_b": [DEPTH, D],
        "w_rt": [DEPTH, D, 36], "b_rt": [DEPTH, 36], "w_gate": [DEPTH, 32, D, 256],
        "w_up": [DEPTH, 32, D, 256], "w_down": [DEPTH, 32, 256, D],
        "t_ident": [128, 128], "t_maskA": [128, 2 * 17 * 4 * 128], "t_maskB": [128, 2 * 2 * 4 * 128],
        "t_maskC": [128, 16 * 4 * 128], "t_gcmp": [128, 8 * 248], "t_inter": [128, 33],
        "t_selmul": [128, NT * 32], "t_seladd": [128, NT * 32], "t_cmul": [128, NT * 32],
        "t_cadd": [128, NT * 32], "t_cown": [128, NT * 32], "t_eselA": [32, NT * 128],
        "t_eselC": [8, NT * 128], "t_onehot": [32, 32 * 128],
    }

    class Lazy(dict):
        def __init__(self, prefix=""):
            super().__init__()
            self.prefix = prefix

        def __missing__(self, key):
            v = din(self.prefix + key, shapes[self.prefix + key])
            self[key] = v
            return v

    I = Lazy()
    T = Lazy("t_")
    out = nc.dram_tensor("out", [TOK, D], F32, kind="ExternalOutput").ap()

    R = {}
    R["xT"] = dscr("s_xT", [KC, 128, TOK], BF16)
    R["qTA"] = dscr("s_qTA", [8, 128, TOK], BF16)
    R["kTA"] = dscr("s_kTA", [3, 2, 128, TOK], BF16)
    R["vTcmp"] = dscr("s_vTcmp", [2, 128, TOK], BF16)
    R["vA"] = dscr("s_vA", [2, TOK, 256], BF16)
    R["gateA"] = dscr("s_gateA", [TOK, 24], F32)
    R["qTB"] = dscr("s_qTB", [8, 64, TOK], BF16)
    R["kTB"] = dscr("s_kTB", [2, 64, TOK], BF16)
    R["vB"] = dscr("s_vB", [TOK, 128], BF16)
    R["qTC"] = dscr("s_qTC", [4, 128, TOK], BF16)
    R["kTC"] = dscr("s_kTC", [4, 128, TOK], BF16)
    R["vC"] = dscr("s_vC", [TOK, 512], BF16)
    R["oTA"] = dscr("s_oTA", [8, 128, TOK], BF16)
    R["oTB"] = dscr("s_oTB", [4, 128, TOK], BF16)
    R["oTC"] = dscr("s_oTC", [4, 128, TOK], BF16)
    R["x1"] = dscr("s_x1", [TOK, D], F32)
    R["x1T"] = dscr("s_x1T", [KC, 128, TOK], BF16)
    R["cT"] = dscr("s_cT", [32, TOK], BF16)
    R["xmid"] = dscr("s_xmid", [TOK, D], F32)
    C.I, C.T, C.R = I, T, R

    C.psS = Ring([S.ps("psS%d" % i, [128, 512]) for i in range(2)])
    C.psV = [S.ps("psV%d" % i, [128, 512]) for i in range(4)]
    C.psX = Ring([S.ps("psX%d" % i, [128, 512]) for i in range(2)])
    C.ident = S.sb("ident", [128, 128], F32)
    C.identb = S.sb("identb", [128, 128], BF16)
    S.dma("sp", C.ident[:], T["ident"], writes=[C.ident])
    C.used = lambda: [k for k in I.keys()] + ["t_" + k for k in T.keys()]
    S.op("dve", lambda: nc.vector.tensor_copy(C.identb[:], C.ident[:]), reads=[C.ident], writes=[C.identb])

    for l in range(n_layers):
        x_in = I["x"] if l == 0 else R["xmid"]
        x_out = out if l == n_layers - 1 else R["xmid"]
        C.l = l
        phase_inproj(C, x_in)
        if "stop_inproj" in dbg:
            break
        phase_mixA(C)
        phase_mixB(C)
        phase_mixC(C)
        if "stop_mix" in dbg:
            break
        for blk in range(2):
            phase_merge(C, x_in, blk)
        if "stop_merge" in dbg:
            break
        for blk in range(2):
            phase_moe(C, blk, x_out)
    S.finish()
    return nc, C.used()


def _evac_engines():
    while True:
        yield "act"
        yield "dve"


def phase_inproj(C, x_in):
    nc, S, I, R = C.nc, C.S, C.I, C.R
    l = C.l
    ev = _evac_engines()
    with S.phase():
        xT = S.sbp("xT", [128, KC, TOK], BF16)
        xparts = [S.tok("xTp%d" % i) for i in range(NT)]
        xin = Ring([S.sbp("xin", [128, D], F32) for _ in range(2)])
        for i in range(NT):
            xb = xin.next()
            S.dma("sp", xb[:], x_in[i * 128:(i + 1) * 128, :], writes=[xb])
            for q in range(4):
                ps = C.psX.next()
                for j in range(4):
                    kc = q * 4 + j
                    S.tr(ps[:, j * 128:(j + 1) * 128], xb[:, kc * 128:(kc + 1) * 128], C.ident[:],
                         reads=[xb, C.ident], writes=[ps])
                S.copy(next(ev), xT[:, q * 4:(q + 1) * 4, i * 128:(i + 1) * 128],
                       ps[:].rearrange("p (a b) -> p a b", a=4), reads=[ps], writes=[xparts[i]])
        for kc in range(KC):
            S.dma("sp", R["xT"][kc], xT[:, kc, :], reads=xparts)

        wbuf = Ring([S.sbp("wbuf", [128, KC, 512], BF16) for _ in range(2)])
        stg = Ring([S.sbp("stg", [128, TOK], BF16) for _ in range(2)])
        stgt = Ring([S.sbp("stgt", [128, 512], BF16) for _ in range(3)])
        stgf = Ring([S.sbp("stgf", [128, 24], F32) for _ in range(2)])
        w_in = I["w_in"]

        def load_w(col0, width):
            wt = wbuf.next()
            src = w_in[l, :, col0:col0 + width].rearrange("(kc p) c -> p kc c", p=128)
            S.dma("pool", wt[:, :, 0:width], src, writes=[wt])
            return wt

        def fm_job(col0, width, sub, dsts):
            wt = load_w(col0, width)
            for j in range(width // sub):
                st = stg.next()
                for tb in range(4):
                    ps = C.psX.next()
                    for kc in range(KC):
                        S.mm(ps[0:sub, :], wt[:, kc, j * sub:(j + 1) * sub], xT[:, kc, tb * 512:(tb + 1) * 512],
                             start=(kc == 0), stop=(kc == KC - 1),
                             reads=[wt] + xparts[tb * 4:(tb + 1) * 4], writes=[ps])
                    S.copy(next(ev), st[0:sub, tb * 512:(tb + 1) * 512], ps[0:sub, :], reads=[ps], writes=[st])
                S.dma("sp", dsts[j], st[0:sub, :], reads=[st])

        def tm_job(col0, width, dst, sigmoid=False):
            wt = load_w(col0, width)
            for i in range(NT):
                ps = C.psX.next()
                for kc in range(KC):
                    S.mm(ps[:, 0:width], xT[:, kc, i * 128:(i + 1) * 128], wt[:, kc, 0:width],
                         start=(kc == 0), stop=(kc == KC - 1), reads=[wt, xparts[i]], writes=[ps])
                if sigmoid:
                    st = stgf.next()
                    S.op("act", lambda: nc.scalar.activation(st[:, 0:width], ps[:, 0:width], AF.Sigmoid),
                         reads=[ps], writes=[st])
                else:
                    st = stgt.next()
                    S.copy(next(ev), st[:, 0:width], ps[:, 0:width], reads=[ps], writes=[st])
                S.dma("sp", dst[i * 128:(i + 1) * 128, :], st[:, 0:width], reads=[st])

        fm_job(OFF_QA, 512, 128, [R["qTA"][h] for h in range(4)])
        fm_job(OFF_QA + 512, 512, 128, [R["qTA"][h] for h in range(4, 8)])
        fm_job(OFF_KVA, 512, 128, [R["kTA"][0, 0], R["kTA"][0, 1], R["vTcmp"][0], R["vTcmp"][1]])
        fm_job(OFF_KVA + 512, 256, 128, [R["kTA"][1, 0], R["kTA"][1, 1]])
        tm_job(OFF_KVA + 768, 256, R["vA"][0])
        fm_job(OFF_KVA + 1024, 256, 128, [R["kTA"][2, 0], R["kTA"][2, 1]])
        tm_job(OFF_KVA + 1280, 256, R["vA"][1])
        tm_job(OFF_GA, 24, R["gateA"], sigmoid=True)
        fm_job(OFF_QB, 512, 64, [R["qTB"][h] for h in range(8)])
        fm_job(OFF_KB, 128, 64, [R["kTB"][g] for g in range(2)])
        tm_job(OFF_VB, 128, R["vB"])
        fm_job(OFF_QC, 512, 128, [R["qTC"][h] for h in range(4)])
        fm_job(OFF_KC_, 512, 128, [R["kTC"][h] for h in range(4)])
        tm_job(OFF_VC, 512, R["vC"])


def _st_attention(C, kts, sel_emit, qk_emit, mask_of, v_of, dv, scale, ebuf, pbuf, par):
    nc, S = C.nc, C.S
    n = len(kts)
    pss = {}

    if not hasattr(C, "psS4"):
        C.psS4 = Ring([C.psS.bufs[0], C.psS.bufs[1], C.psX.bufs[0], C.psX.bufs[1]])
    LOOK = 3

    def emit_scores(idx):
        ps = C.psS4.next()
        first = True
        if sel_emit is not None:
            sel_emit(ps, kts[idx])
            first = False
        qk_emit(ps, kts[idx], first)
        pss[idx] = ps

    for j in range(min(LOOK, n)):
        emit_scores(j)
    for idx, kt in enumerate(kts):
        if idx + LOOK < n:
            emit_scores(idx + LOOK)
        ps = pss.pop(idx)
        E = ebuf.next()
        S.op("act", lambda: nc.scalar.activation(E[:], ps[:], AF.Exp, scale=scale), reads=[ps], writes=[E])
        P = pbuf.next()
        mk, mtok = mask_of(kt)
        eng = "dve"
        par[0] += 1
        h = nc.gpsimd if eng == "pool" else nc.vector
        S.op(eng, lambda: h.tensor_tensor(out=P[:].rearrange("p (a b) -> p a b", a=4),
                                          in0=E[:].rearrange("p (a b) -> p a b", a=4), in1=mk, op=ALU.mult),
             reads=[E, mtok], writes=[P])
        for hh in range(4):
            va, vtok = v_of(kt, hh)
            S.mm(C.psV[hh][:, 0:dv + 1], P[:, hh * 128:(hh + 1) * 128], va,
                 start=(idx == 0), stop=(idx == n - 1), reads=[P, vtok], writes=[C.psV[hh]])
    _flush_pending(C)


def _flush_pending(C):
    p = getattr(C, "pending", None)
    C.pending = None
    if p is not None:
        p()


def _post_bundle(C, dv, dsts, gates, first, small, extras=None, defer=False):
    nc, S = C.nc, C.S
    ncol = C.pvcols
    pvs, dens = [], []
    for hh in range(4):
        pvp = C.psV[hh]
        pv = C.pvbuf.next()
        S.op("act", lambda: nc.scalar.copy(pv[:, 0:ncol], pvp[:, 0:ncol]), reads=[pvp], writes=[pv])
        pvs.append(pv)
        dens.append(small.next())
    if defer:
        _flush_pending(C)
        C.pending = lambda: _post_chain(C, dv, dsts, gates, first, extras, pvs, dens)
        return dens, pvs
    _post_chain(C, dv, dsts, gates, first, extras, pvs, dens)
    return dens, pvs


def _post_chain(C, dv, dsts, gates, first, extras, pvs, dens):
    nc, S = C.nc, C.S
    for hh in range(4):
        pv, den = pvs[hh], dens[hh]
        if extras is not None:
            ex, extok = extras[hh]
            S.op("dve", lambda: nc.vector.tensor_tensor(out=den[:, 0:1], in0=pv[:, dv:dv + 1], in1=ex, op=ALU.add),
                 reads=[pv, extok], writes=[den])
        else:
            S.op("dve", lambda: nc.vector.tensor_scalar(out=den[:, 0:1], in0=pv[:, dv:dv + 1], scalar1=1e-30,
                                                        scalar2=None, op0=ALU.max), reads=[pv], writes=[den])
    for hh in range(4):
        den = dens[hh]
        S.op("dve", lambda: nc.vector.reciprocal(den[:, 1:2], den[:, 0:1]), reads=[den], writes=[den])
    ws = []
    for hh in range(4):
        den = dens[hh]
        if gates is not None:
            gap, gtok = gates[hh]
            S.op("dve", lambda: nc.vector.tensor_tensor(out=den[:, 2:3], in0=den[:, 1:2], in1=gap, op=ALU.mult),
                 reads=[den, gtok], writes=[den])
            ws.append(den[:, 2:3])
        else:
            ws.append(den[:, 1:2])
    for hh in range(4):
        pv, den, w = pvs[hh], dens[hh], ws[hh]
        dap, dtok = dsts[hh]
        if first:
            S.op("dve", lambda: nc.vector.tensor_scalar(out=dap, in0=pv[:, 0:dv], scalar1=w, scalar2=None,
                                                        op0=ALU.mult), reads=[pv, den], writes=[dtok])
        else:
            S.op("dve", lambda: nc.vector.scalar_tensor_tensor(out=dap, in0=pv[:, 0:dv], scalar=w, in1=dap,
                                                               op0=ALU.mult, op1=ALU.add),
                 reads=[pv, den, dtok], writes=[dtok])
    return dens, pvs


def _finish_tile(C, oacc, nchunk, ob, ost, dst, i, ev):
    nc, S = C.nc, C.S
    _flush_pending(C)
    S.op("act", lambda: nc.scalar.copy(ob[:, 0:nchunk * 128], oacc[:].rearrange("p a b -> p (a b)")),
         reads=[oacc], writes=[ob])
    ps = C.psX.next()
    psb = ps[:].bitcast(BF16)
    for c in range(nchunk):
        S.tr(psb[:, c * 128:(c + 1) * 128], ob[:, c * 128:(c + 1) * 128], C.identb[:],
             reads=[ob, C.identb], writes=[ps])
    S.copy(next(ev), ost[:, 0:nchunk * 128], psb[:, 0:nchunk * 128], reads=[ps], writes=[ost])
    S.dma("sp", dst[:, :, i * 128:(i + 1) * 128].rearrange("c p t -> p c t"),
          ost[:, 0:nchunk * 128].rearrange("p (c t) -> p c t", c=nchunk), reads=[ost])


def phase_mixA(C):
    nc, S, I, R, T = C.nc, C.S, C.I, C.R, C.T
    l = C.l
    ev = _evac_engines()
    par = [0]
    with S.phase():
        MA = S.sbp("MA", [128, 2, 17, 4, 128], BF16)
        for g in range(2):
            S.dma("pool", MA[:, g].rearrange("p a b c -> p (a b) c"),
                  T["maskA"][:, g * 8704:(g + 1) * 8704].rearrange("p (a c) -> p a c", c=128), writes=[MA])
        Gc = S.sbp("Gc", [128, 8, 248], BF16)
        S.dma("pool", Gc[:], T["gcmp"].rearrange("p (a c) -> p a c", c=248), writes=[Gc])
        kT = S.sbp("kT", [128, 2, 2, TOK], BF16)
        for b in range(2):
            for g in range(2):
                S.dma("sp", kT[:, b, g, :], R["kTA"][1 + b, g], writes=[kT])
        vaug = S.sbp("vaug", [128, 2, 2, NT, 129], BF16)
        S.op("pool", lambda: nc.gpsimd.memset(vaug[:], 1.0), writes=[vaug])
        for b in range(2):
            for g in range(2):
                S.dma("sp", vaug[:, b, g, :, 0:128],
                      R["vA"][b][:, g * 128:(g + 1) * 128].rearrange("(kt p) d -> p kt d", p=128), writes=[vaug])
        gt = S.sbp("gt", [128, NT, 24], F32)
        S.dma("sp", gt[:], R["gateA"].rearrange("(i p) c -> p i c", p=128), writes=[gt])
        selmul = S.sbp("selmul", [128, NT, 32], F32)
        seladd = S.sbp("seladd", [128, NT, 32], F32)
        S.dma("sp", selmul[:], T["selmul"].rearrange("p (i c) -> p i c", c=32), writes=[selmul])
        S.dma("sp", seladd[:], T["seladd"].rearrange("p (i c) -> p i c", c=32), writes=[seladd])
        esel = S.sbp("esel", [128, NT, 128], BF16)
        S.op("pool", lambda: nc.gpsimd.memset(esel[:], 0.0), writes=[esel])
        S.dma("pool", esel[0:32], T["eselA"].rearrange("p (i c) -> p i c", c=128), writes=[esel])

        kcin = S.sbp("kcin", [128, 2, TOK], BF16)
        vcin = S.sbp("vcin", [128, 2, TOK], BF16)
        for g in range(2):
            S.dma("sp", kcin[:, g, :], R["kTA"][0, g], writes=[kcin])
            S.dma("sp", vcin[:, g, :], R["vTcmp"][g], writes=[vcin])
        cw = S.sbp("cw", [128, 2, 32, 128], BF16)
        for k in range(2):
            S.dma("pool", cw[:, k], I["nsa_cmp_w"][l, k].rearrange("l d e -> d l e"), writes=[cw])
        posin = S.sbp("posin", [64, 128], F32)
        S.dma("sp", posin[:], I["nsa_cmp_pos"][l], writes=[posin])
        posT = S.sbp("posT", [128, 64], BF16)
        ps = C.psX.next()
        S.tr(ps[:, 0:64], posin[:], C.ident[0:64, 0:64], reads=[posin, C.ident], writes=[ps])
        S.copy("dve", posT[:], ps[:, 0:64], reads=[ps], writes=[posT])
        ck = S.sbp("ck", [128, 2], F32)
        ps = C.psX.next()
        for kv in range(2):
            for li in range(32):
                S.mm(ps[:, kv:kv + 1], cw[:, kv, li, :], posT[:, kv * 32 + li:kv * 32 + li + 1],
                     start=(li == 0), stop=(li == 31), reads=[cw, posT], writes=[ps])
        S.copy("dve", ck[:], ps[:, 0:2], reads=[ps], writes=[ck])
        kcmpT = S.sbp("kcmpT", [128, 2, 128], BF16)
        vcaug = S.sbp("vcaug", [128, 2, 161], BF16)
        vtmp = S.sbp("vtmp", [128, 128], BF16)
        S.op("dve", lambda: nc.vector.memset(kcmpT[:], 0.0), writes=[kcmpT])
        S.op("dve", lambda: nc.vector.memset(vtmp[:], 0.0), writes=[vtmp])
        for g in range(2):
            S.dma("pool", vcaug[:, g, 128:161], T["inter"], writes=[vcaug])
            for kv in range(2):
                src = kcin if kv == 0 else vcin
                ps = C.psX.next()
                for li in range(32):
                    S.mm(ps[:, 0:127], cw[:, kv, li, :], src[:, g, li:li + 2017:16],
                         start=(li == 0), stop=(li == 31), reads=[cw, src], writes=[ps])
                if kv == 0:
                    S.op("dve", lambda: nc.vector.tensor_scalar(out=kcmpT[:, g, 0:127], in0=ps[:, 0:127],
                                                                scalar1=ck[:, 0:1], scalar2=None, op0=ALU.add),
                         reads=[ps, ck], writes=[kcmpT])
                else:
                    S.op("dve", lambda: nc.vector.tensor_scalar(out=vtmp[:, 0:127], in0=ps[:, 0:127],
                                                                scalar1=ck[:, 1:2], scalar2=None, op0=ALU.add),
                         reads=[ps, ck], writes=[vtmp])
                    ps2 = C.psX.next()
                    psb = ps2[:].bitcast(BF16)
                    S.tr(psb[:, 0:128], vtmp[:], C.identb[:], reads=[vtmp, C.identb], writes=[ps2])
                    S.copy("dve", vcaug[:, g, 0:128], psb[:, 0:128], reads=[ps2], writes=[vcaug])

        qbuf = Ring([S.sbp("qA", [128, 8, 128], BF16) for _ in range(2)])
        ebuf = Ring([S.sbp("E", [128, 512], BF16) for _ in range(4)])
        pbuf = Ring([S.sbp("P", [128, 512], BF16) for _ in range(4)])
        ptb = Ring([S.sbp("PT", [128, 128], BF16) for _ in range(3)])
        small = Ring([S.sbp("sm", [128, 4], F32) for _ in range(8)])
        C.pvbuf = Ring([S.sbp("pvb", [128, 161], F32) for _ in range(8)])
        C.pvcols = 129
        oaccs = Ring([S.sbp("oacc", [128, 8, 128], F32) for _ in range(2)])
        imps = Ring([S.sbp("imp", [128, 2, 32], F32) for _ in range(2)])
        impf = Ring([S.sbp("impf", [128, 32], F32) for _ in range(2)])
        m8 = Ring([S.sbp("m8", [128, 8], F32) for _ in range(2)])
        selb = Ring([S.sbp("selb", [128, 128], F32) for _ in range(2)])
        for b_ in selb.bufs:
            S.op("dve", lambda: nc.vector.memset(b_[:], 0.0), writes=[b_])
        selbT = Ring([S.sbp("selbT", [128, 2, 4, 128], BF16) for _ in range(2)])
        for b_ in selbT.bufs:
            S.op("pool", lambda: nc.gpsimd.memset(b_[:], 0.0), writes=[b_])
        obs = Ring([S.sbp("ob", [128, 1024], BF16) for _ in range(2)])
        osts = Ring([S.sbp("ost", [128, 1024], BF16) for _ in range(2)])

        st_ = {}

        def prep(i):
            qt = qbuf.next()
            S.dma("sp", qt[:], R["qTA"][:, :, i * 128:(i + 1) * 128].rearrange("h p t -> p h t"), writes=[qt])
            oacc = oaccs.next()
            imp = imps.next()
            sT = selbT.next()
            off = 120 - 8 * i
            st_[i] = (qt, oacc, imp, sT, off)

        def cmp_g(i, g):
            qt, oacc, imp, sT, off = st_[i]
            ps = C.psS.next()
            for hh in range(4):
                S.mm(ps[:, hh * 128:(hh + 1) * 128], qt[:, 4 * g + hh, :], kcmpT[:, g, :],
                     reads=[qt, kcmpT], writes=[ps])
            E = ebuf.next()
            S.op("act", lambda: nc.scalar.activation(E[:], ps[:], AF.Exp, scale=SC_A), reads=[ps], writes=[E])
            P = pbuf.next()
            S.op("pool", lambda: nc.gpsimd.tensor_tensor(
                out=P[:].rearrange("p (a b) -> p a b", a=4), in0=E[:].rearrange("p (a b) -> p a b", a=4),
                in1=Gc[:, 4 * g:4 * g + 4, off:off + 128], op=ALU.mult), reads=[E, Gc], writes=[P])
            for hh in range(4):
                pst = C.psX.next()
                pstb = pst[:].bitcast(BF16)
                S.tr(pstb[:, 0:128], P[:, hh * 128:(hh + 1) * 128], C.identb[:], reads=[P, C.identb], writes=[pst])
                PT = ptb.next()
                S.copy(next(ev), PT[:], pstb[:, 0:128], reads=[pst], writes=[PT])
                S.mm(C.psV[hh][:, 0:161], PT[:], vcaug[:, g, :], reads=[PT, vcaug], writes=[C.psV[hh]])
            C.pvcols = 161
            dens, pvs = _post_bundle(C, 128, [(oacc[:, 4 * g + hh, :], oacc) for hh in range(4)],
                                     [(gt[:, i, 3 * (4 * g + hh):3 * (4 * g + hh) + 1], gt) for hh in range(4)],
                                     True, small)
            C.pvcols = 129
            for hh in range(4):
                h = 4 * g + hh
                den, pv = dens[hh], pvs[hh]
                if hh == 0:
                    S.op("dve", lambda: nc.vector.tensor_scalar(out=imp[:, g, :], in0=pv[:, 129:161],
                                                                scalar1=den[:, 1:2], scalar2=None, op0=ALU.mult),
                         reads=[pv, den], writes=[imp])
                else:
                    S.op("dve", lambda: nc.vector.scalar_tensor_tensor(
                        out=imp[:, g, :], in0=pv[:, 129:161], scalar=den[:, 1:2], in1=imp[:, g, :],
                        op0=ALU.mult, op1=ALU.add), reads=[pv, den, imp], writes=[imp])

        def sel_g(i, g):
            qt, oacc, imp, sT, off = st_[i]
            f = impf.next()
            S.op("dve", lambda: nc.vector.tensor_tensor(out=f[:], in0=imp[:, g, :], in1=selmul[:, i, :], op=ALU.mult),
                 reads=[imp, selmul], writes=[f])
            S.op("dve", lambda: nc.vector.tensor_tensor(out=f[:], in0=f[:], in1=seladd[:, i, :], op=ALU.add),
                 reads=[f, seladd], writes=[f])
            m = m8.next()
            S.op("dve", lambda: nc.vector.max(out=m[:], in_=f[:]), reads=[f], writes=[m])
            sb_ = selb.next()
            S.op("dve", lambda: nc.vector.tensor_scalar(out=sb_[:, 0:32], in0=f[:], scalar1=m[:, 7:8], scalar2=None,
                                                        op0=ALU.is_ge), reads=[f, m], writes=[sb_])
            S.op("dve", lambda: nc.vector.tensor_scalar(out=sb_[:, 0:32], in0=sb_[:, 0:32], scalar1=-1.0, scalar2=NEGB,
                                                        op0=ALU.add, op1=ALU.mult), reads=[sb_], writes=[sb_])
            pst = C.psX.next()
            S.tr(pst[:, 0:128], sb_[:], C.ident[:], reads=[sb_, C.ident], writes=[pst])
            for hh in range(4):
                S.copy(next(ev), sT[0:32, g, hh, :], pst[0:32, 0:128], reads=[pst], writes=[sT])

        def bundle(i, br, g):
            qt, oacc, imp, sT, off = st_[i]
            if br == 0:
                kts = list(range(0, i + 1))
                sel_emit = (lambda ps, kt, g=g: S.mm(
                    ps[:], esel[:, kt, :], sT[:, g].rearrange("p a b -> p (a b)"), start=True, stop=False,
                    reads=[esel, sT], writes=[ps]))
                mask_of = (lambda kt, g=g: (MA[:, g, i - kt], MA))
            else:
                kts = list(range(max(0, i - 4), i + 1))
                sel_emit = None
                mask_of = (lambda kt, g=g: (MA[:, g, (i - kt) if (i - kt) < 4 else 16], MA))
            qk_emit = (lambda ps, kt, first, g=g, br=br: S.mm(
                ps[:].rearrange("p (a b) -> p a b", a=4), kT[:, br, g, kt * 128:(kt + 1) * 128],
                qt[:, 4 * g:4 * g + 4, :], start=first, stop=True, reads=[kT, qt], writes=[ps]))
            v_of = (lambda kt, hh, g=g, br=br: (vaug[:, br, g, kt, :], vaug))
            _st_attention(C, kts, sel_emit, qk_emit, mask_of, v_of, 128, SC_A, ebuf, pbuf, par)
            _post_bundle(C, 128, [(oacc[:, 4 * g + hh, :], oacc) for hh in range(4)],
                         [(gt[:, i, 3 * (4 * g + hh) + 1 + br:3 * (4 * g + hh) + 2 + br], gt) for hh in range(4)],
                         False, small, defer=True)

        def finish(i):
            qt, oacc, imp, sT, off = st_.pop(i)
            _finish_tile(C, oacc, 8, obs.next(), osts.next(), R["oTA"], i, ev)

        prep(0)
        for g in range(2):
            cmp_g(0, g)
        for g in range(2):
            sel_g(0, g)
        for i in range(NT):
            nx = i + 1 < NT
            if nx:
                prep(i + 1)
            bundle(i, 0, 0)
            if nx:
                cmp_g(i + 1, 0)
            bundle(i, 0, 1)
            if nx:
                cmp_g(i + 1, 1)
            bundle(i, 1, 0)
            if nx:
                sel_g(i + 1, 0)
                sel_g(i + 1, 1)
            bundle(i, 1, 1)
            finish(i)


def _tables():
    T = {}
    T["t_ident"] = np.eye(128, dtype=np.float32)
    sp = np.arange(128)[:, None]
    tp = np.arange(128)[None, :]
    slA = np.array([2.0 ** (-(h + 1)) for h in range(8)])
    slC = np.array([2.0 ** (-2.0 * (h + 1)) for h in range(4)])

    def toep(slope, delta, wcut=None):
        u = (delta + tp - sp).astype(np.float64)
        v = np.exp(-slope * np.maximum(u, 0.0))
        v = np.where(u >= 0, v, 0.0)
        if wcut is not None:
            v = np.where(u < wcut, v, 0.0)
        return v

    mA = np.zeros((128, 2, 17, 4, 128), np.float64)
    for g in range(2):
        for hh in range(4):
            s = slA[4 * g + hh]
            for di in range(16):
                mA[:, g, di, hh, :] = toep(s, 128 * di)
            mA[:, g, 16, hh, :] = toep(s, 512, 512)
    T["t_maskA"] = mA.reshape(128, -1).astype(np.float32)
    mB = np.zeros((128, 2, 2, 4, 128), np.float64)
    for g in range(2):
        for hh in range(4):
            s = slA[4 * g + hh]
            mB[:, g, 0, hh, :] = toep(s, 0, 128)
            mB[:, g, 1, hh, :] = toep(s, 128, 128)
    T["t_maskB"] = mB.reshape(128, -1).astype(np.float32)
    mC = np.zeros((128, 16, 4, 128), np.float64)
    for di in range(16):
        for h in range(4):
            mC[:, di, h, :] = toep(slC[h], 128 * di)
    T["t_maskC"] = mC.reshape(128, -1).astype(np.float32)
    tq = np.arange(128)[:, None]
    m = (np.arange(248) - 120)[None, :]
    dist = (tq - 16 * m - 31).astype(np.float64)
    G = np.zeros((128, 8, 248), np.float64)
    for h in range(8):
        G[:, h, :] = np.where(dist >= 0, np.exp(-slA[h] * np.maximum(dist, 0.0)), 0.0)
    T["t_gcmp"] = G.reshape(128, -1).astype(np.float32)
    n_cmp = 127
    c_start = 16 * np.arange(n_cmp)
    s_start = 64 * np.arange(32)
    inter = np.clip(np.minimum(c_start[:, None] + 32, s_start[None, :] + 64)
                    - np.maximum(c_start[:, None], s_start[None, :]), 0, None) / 32.0
    ia = np.zeros((128, 33), np.float32)
    ia[:127, 0] = 1.0
    ia[:127, 1:] = inter
    T["t_inter"] = ia
    t = (128 * np.arange(NT)[None, :, None] + np.arange(128)[:, None, None])
    j = np.arange(32)[None, None, :]
    blk = t // 64
    forced = (j == 0) | (j == blk) | (j == blk - 1)
    valid = j <= blk
    T["t_selmul"] = (valid & ~forced).astype(np.float32).reshape(128, -1)
    T["t_seladd"] = np.where(forced, BIG, np.where(valid, 0.0, -BIG)).astype(np.float32).reshape(128, -1)
    n8 = np.arange(8)[None, None, None, :]
    t4 = t[:, :, :, None] + np.zeros((1, 1, 4, 1), np.int64)
    blk_c = t4 // 256
    past = n8 < blk_c
    T["t_cmul"] = past.astype(np.float32).reshape(128, -1)
    T["t_cadd"] = np.where(past, 0.0, -BIG).astype(np.float32).reshape(128, -1)
    T["t_cown"] = (n8 == blk_c).astype(np.float32).reshape(128, -1)
    eA = np.zeros((32, NT, 128), np.float32)
    for kt in range(NT):
        for s in range(128):
            eA[2 * kt + s // 64, kt, s] = 1.0
    T["t_eselA"] = eA.reshape(32, -1)
    eC = np.zeros((8, NT, 128), np.float32)
    for kt in range(NT):
        eC[kt // 2, kt, :] = 1.0
    T["t_eselC"] = eC.reshape(8, -1)
    oh = np.zeros((32, 32, 128), np.float32)
    for e in range(32):
        oh[e, e, :] = 1.0
    T["t_onehot"] = oh.reshape(32, -1)
    return T


def _prep_inputs(inputs):
    f = lambda a: np.ascontiguousarray(np.asarray(a, dtype=np.float32))
    common = {}
    for nm in ("w_in", "nsa_cmp_w", "sink_b", "w_br_a", "w_br_b", "w_br_c", "w_out",
               "ln1_g", "ln1_b", "ln2_g", "ln2_b", "w_gate", "w_up", "w_down"):
        common[nm] = f(inputs[nm])
    common["nsa_cmp_pos"] = f(inputs["nsa_cmp_pos"]).reshape(DEPTH, 64, 128)
    common["w_rt"] = np.ascontiguousarray(np.concatenate([f(inputs["w_group"]), f(inputs["w_router"])], axis=-1))
    common["b_rt"] = np.ascontiguousarray(np.concatenate([f(inputs["b_group"]), f(inputs["b_router"])], axis=-1))
    common.update(_tables())
    x = f(inputs["x"])
    in_maps = []
    for c in range(8):
        d = dict(common)
        d["x"] = np.ascontiguousarray(x[c % 4])
        in_maps.append(d)
    return in_maps


def kernel(**inputs):
    nc, used = build_program()
    in_maps = [{k: m[k] for k in used} for m in _prep_inputs(inputs)]
    res = run_bass_kernel_spmd(nc, in_maps, core_ids=list(range(8)))
    out = np.stack([np.asarray(res.results[c]["out"], dtype=np.float32) for c in range(4)], axis=0)
    return out


def phase_mixB(C):
    nc, S, I, R, T = C.nc, C.S, C.I, C.R, C.T
    l = C.l
    ev = _evac_engines()
    par = [0]
    with S.phase():
        MB = S.sbp("MB", [128, 2, 2, 4, 128], BF16)
        S.dma("pool", MB[:].rearrange("p g a b c -> p (g a b) c"),
              T["maskB"].rearrange("p (a c) -> p a c", c=128), writes=[MB])
        kT = S.sbp("kTB", [128, 2, TOK], BF16)
        S.op("pool", lambda: nc.gpsimd.memset(kT[:], 0.0), writes=[kT])
        for g in range(2):
            S.dma("sp", kT[0:64, g, :], R["kTB"][g], writes=[kT])
        vaug = S.sbp("vaugB", [128, 2, NT, 65], BF16)
        S.op("pool", lambda: nc.gpsimd.memset(vaug[:], 1.0), writes=[vaug])
        for g in range(2):
            S.dma("sp", vaug[:, g, :, 0:64],
                  R["vB"][:, g * 64:(g + 1) * 64].rearrange("(kt p) d -> p kt d", p=128), writes=[vaug])
        snk = S.sbp("snk", [128, 8], F32)
        S.dma("sp", snk[:], I["sink_b"][l].partition_broadcast(128), writes=[snk])
        esnk = S.sbp("esnk", [128, 8], F32)
        S.op("act", lambda: nc.scalar.activation(esnk[:], snk[:], AF.Exp), reads=[snk], writes=[esnk])
        qbuf = Ring([S.sbp("qB", [128, 8, 128], BF16) for _ in range(2)])
        for b_ in qbuf.bufs:
            S.op("pool", lambda: nc.gpsimd.memset(b_[:], 0.0), writes=[b_])
        ebuf = Ring([S.sbp("E", [128, 512], BF16) for _ in range(4)])
        pbuf = Ring([S.sbp("P", [128, 512], BF16) for _ in range(4)])
        small = Ring([S.sbp("sm", [128, 4], F32) for _ in range(8)])
        C.pvbuf = Ring([S.sbp("pvb", [128, 161], F32) for _ in range(8)])
        C.pvcols = 65
        oaccs = Ring([S.sbp("oaccB", [128, 8, 64], F32) for _ in range(2)])
        obs = Ring([S.sbp("ob", [128, 512], BF16) for _ in range(2)])
        osts = Ring([S.sbp("ost", [128, 512], BF16) for _ in range(2)])
        for i in range(NT):
            qt = qbuf.next()
            S.dma("sp", qt[0:64], R["qTB"][:, :, i * 128:(i + 1) * 128].rearrange("h p t -> p h t"), writes=[qt])
            oacc = oaccs.next()
            for g in range(2):
                kts = list(range(max(0, i - 1), i + 1))
                qk_emit = (lambda ps, kt, first, g=g: S.mm(
                    ps[:].rearrange("p (a b) -> p a b", a=4), kT[:, g, kt * 128:(kt + 1) * 128],
                    qt[:, 4 * g:4 * g + 4, :], start=first, stop=True, reads=[kT, qt], writes=[ps]))
                mask_of = (lambda kt, g=g: (MB[:, g, i - kt], MB))
                v_of = (lambda kt, hh, g=g: (vaug[:, g, kt, :], vaug))
                _st_attention(C, kts, None, qk_emit, mask_of, v_of, 64, SC_B, ebuf, pbuf, par)
                _post_bundle(C, 64, [(oacc[:, 4 * g + hh, :], oacc) for hh in range(4)], None, True, small,
                             extras=[(esnk[:, 4 * g + hh:4 * g + hh + 1], esnk) for hh in range(4)], defer=True)
            _finish_tile(C, oacc, 4, obs.next(), osts.next(), R["oTB"], i, ev)


def phase_mixC(C):
    nc, S, I, R, T = C.nc, C.S, C.I, C.R, C.T
    ev = _evac_engines()
    par = [0]
    with S.phase():
        MC = S.sbp("MC", [128, 16, 4, 128], BF16)
        S.dma("pool", MC[:].rearrange("p a b c -> p (a b) c"),
              T["maskC"].rearrange("p (a c) -> p a c", c=128), writes=[MC])
        kT = S.sbp("kTC", [128, 4, TOK], BF16)
        for h in range(4):
            S.dma("sp", kT[:, h, :], R["kTC"][h], writes=[kT])
        vaug = S.sbp("vaugC", [128, 4, NT, 129], BF16)
        S.op("pool", lambda: nc.gpsimd.memset(vaug[:], 1.0), writes=[vaug])
        for h in range(4):
            S.dma("sp", vaug[:, h, :, 0:128],
                  R["vC"][:, h * 128:(h + 1) * 128].rearrange("(kt p) d -> p kt d", p=128), writes=[vaug])
        cmul = S.sbp("cmul", [128, NT, 32], F32)
        cadd = S.sbp("cadd", [128, NT, 32], F32)
        cown = S.sbp("cown", [128, NT, 32], F32)
        for t_, nm in ((cmul, "cmul"), (cadd, "cadd"), (cown, "cown")):
            S.dma("sp", t_[:], T[nm].rearrange("p (i c) -> p i c", c=32), writes=[t_])
        esel = S.sbp("eselC", [128, NT, 128], BF16)
        S.op("pool", lambda: nc.gpsimd.memset(esel[:], 0.0), writes=[esel])
        S.dma("pool", esel[0:8], T["eselC"].rearrange("p (i c) -> p i c", c=128), writes=[esel])
        kmf = S.sbp("kmf", [128, 4, 8], F32)
        for h in range(4):
            S.op("dve", lambda: nc.vector.tensor_reduce(
                out=kmf[:, h, :], in_=kT[:, h, :].rearrange("p (n s) -> p n s", s=256), axis=AX.X, op=ALU.add),
                reads=[kT], writes=[kmf])
        kmT = S.sbp("kmT", [128, 4, 8], BF16)
        S.op("dve", lambda: nc.vector.tensor_scalar(out=kmT[:], in0=kmf[:], scalar1=1.0 / 256.0, scalar2=None,
                                                    op0=ALU.mult), reads=[kmf], writes=[kmT])
        qbuf = Ring([S.sbp("qC", [128, 4, 128], BF16) for _ in range(2)])
        ebuf = Ring([S.sbp("E", [128, 512], BF16) for _ in range(4)])
        pbuf = Ring([S.sbp("P", [128, 512], BF16) for _ in range(4)])
        small = Ring([S.sbp("sm", [128, 4], F32) for _ in range(8)])
        C.pvbuf = Ring([S.sbp("pvb", [128, 161], F32) for _ in range(8)])
        C.pvcols = 129
        oaccs = Ring([S.sbp("oaccC", [128, 4, 128], F32) for _ in range(2)])
        scf = Ring([S.sbp("scf", [128, 32], F32) for _ in range(2)])
        m8 = Ring([S.sbp("m8c", [128, 4, 8], F32) for _ in range(2)])
        selb = Ring([S.sbp("selbc", [128, 32], F32) for _ in range(2)])
        selbT = Ring([S.sbp("selbTc", [128, 4, 128], BF16) for _ in range(2)])
        for b_ in selbT.bufs:
            S.op("pool", lambda: nc.gpsimd.memset(b_[:], 0.0), writes=[b_])
        obs = Ring([S.sbp("ob", [128, 512], BF16) for _ in range(2)])
        osts = Ring([S.sbp("ost", [128, 512], BF16) for _ in range(2)])
        for i in range(NT):
            qt = qbuf.next()
            S.dma("sp", qt[:], R["qTC"][:, :, i * 128:(i + 1) * 128].rearrange("h p t -> p h t"), writes=[qt])
            oacc = oaccs.next()
            ps = C.psX.next()
            for h in range(4):
                S.mm(ps[:, h * 8:(h + 1) * 8], qt[:, h, :], kmT[:, h, :], reads=[qt, kmT], writes=[ps])
            f = scf.next()
            S.op("dve", lambda: nc.vector.tensor_tensor(out=f[:], in0=ps[:, 0:32], in1=cmul[:, i, :], op=ALU.mult),
                 reads=[ps, cmul], writes=[f])
            S.op("dve", lambda: nc.vector.tensor_tensor(out=f[:], in0=f[:], in1=cadd[:, i, :], op=ALU.add),
                 reads=[f, cadd], writes=[f])
            m = m8.next()
            sb_ = selb.next()
            for h in range(4):
                S.op("dve", lambda: nc.vector.max(out=m[:, h, :], in_=f[:, h * 8:(h + 1) * 8]), reads=[f], writes=[m])
                S.op("dve", lambda: nc.vector.tensor_scalar(out=sb_[:, h * 8:(h + 1) * 8], in0=f[:, h * 8:(h + 1) * 8],
                                                            scalar1=m[:, h, 2:3], scalar2=None, op0=ALU.is_ge),
                     reads=[f, m], writes=[sb_])
            S.op("dve", lambda: nc.vector.tensor_tensor(out=sb_[:], in0=sb_[:], in1=cown[:, i, :], op=ALU.max),
                 reads=[sb_, cown], writes=[sb_])
            S.op("dve", lambda: nc.vector.tensor_scalar(out=sb_[:], in0=sb_[:], scalar1=-1.0, scalar2=NEGB,
                                                        op0=ALU.add, op1=ALU.mult), reads=[sb_], writes=[sb_])
            pst = C.psX.next()
            for h in range(4):
                S.tr(pst[0:8, h * 128:(h + 1) * 128], sb_[:, h * 8:(h + 1) * 8], C.ident[:],
                     reads=[sb_, C.ident], writes=[pst])
            sT = selbT.next()
            S.copy(next(ev), sT[0:8].rearrange("p a b -> p (a b)"), pst[0:8, :], reads=[pst], writes=[sT])
            kts = list(range(0, i + 1))
            sel_emit = (lambda ps, kt: S.mm(ps[:], esel[:, kt, :], sT[:].rearrange("p a b -> p (a b)"),
                                            start=True, stop=False, reads=[esel, sT], writes=[ps]))

            def qk_emit(ps, kt, first):
                for h in range(4):
                    S.mm(ps[:, h * 128:(h + 1) * 128], kT[:, h, kt * 128:(kt + 1) * 128], qt[:, h, :],
                         start=False, stop=(h == 3), reads=[kT, qt], writes=[ps])
            mask_of = (lambda kt: (MC[:, i - kt], MC))
            v_of = (lambda kt, hh: (vaug[:, hh, kt, :], vaug))
            _st_attention(C, kts, sel_emit, qk_emit, mask_of, v_of, 128, SC_A, ebuf, pbuf, par)
            _post_bundle(C, 128, [(oacc[:, hh, :], oacc) for hh in range(4)], None, True, small, defer=True)
            _finish_tile(C, oacc, 4, obs.next(), osts.next(), R["oTC"], i, ev)


def _layernorm_tile(C, y, g_sb, b_sb, xn, out_t, st, mv, part=0):
    nc, S = C.nc, C.S
    for c in range(4):
        S.op("dve", lambda: nc.vector.bn_stats(st[:, c, :], y[:, c * 512:(c + 1) * 512]), reads=[y], writes=[st])
    S.op("dve", lambda: nc.vector.bn_aggr(mv[:, 0:2], st[:].rearrange("p a b -> p (a b)")), reads=[st], writes=[mv])
    S.op("dve", lambda: nc.vector.tensor_scalar(out=mv[:, 2:3], in0=mv[:, 1:2], scalar1=LN_EPS, scalar2=None,
                                                op0=ALU.add), reads=[mv], writes=[mv])
    S.op("act", lambda: nc.scalar.activation(mv[:, 3:4], mv[:, 2:3], AF.Sqrt), reads=[mv], writes=[mv])
    if part == 1:
        return
    _ln_part2(C, y, g_sb, b_sb, xn, out_t, mv)


def _ln_part2(C, y, g_sb, b_sb, xn, out_t, mv):
    nc, S = C.nc, C.S
    S.op("dve", lambda: nc.vector.reciprocal(mv[:, 4:5], mv[:, 3:4]), reads=[mv], writes=[mv])
    S.op("dve", lambda: nc.vector.tensor_scalar(out=mv[:, 5:6], in0=mv[:, 0:1], scalar1=-1.0, scalar2=mv[:, 4:5],
                                                op0=ALU.mult, op1=ALU.mult), reads=[mv], writes=[mv])
    S.op("act", lambda: nc.scalar.activation(xn[:], y[:], AF.Identity, bias=mv[:, 5:6], scale=mv[:, 4:5]),
         reads=[y, mv], writes=[xn])
    S.op("dve", lambda: nc.vector.tensor_tensor(out=xn[:], in0=xn[:], in1=g_sb[:], op=ALU.mult),
         reads=[xn, g_sb], writes=[xn])
    S.op("pool", lambda: nc.gpsimd.tensor_tensor(out=out_t[:], in0=xn[:], in1=b_sb[:], op=ALU.add),
         reads=[xn, b_sb], writes=[out_t])


def phase_merge(C, x_in, blk):
    nc, S, I, R, T = C.nc, C.S, C.I, C.R, C.T
    l = C.l
    ev = _evac_engines()
    t0 = blk * 1024
    with S.phase():
        mT = S.sbp("mT", [128, KC, 1024], BF16)
        wo = S.sbp("wo", [128, KC, D], BF16)

        def load_wo():
            for db in range(4):
                S.dma("pool", wo[:, :, db * 512:(db + 1) * 512],
                      I["w_out"][l, :, db * 512:(db + 1) * 512].rearrange("(kc p) c -> p kc c", p=128), writes=[wo])

        with S.phase():
            xT = S.sbp("xTm", [128, KC, 1024], BF16)
            S.dma("sp", xT[:], R["xT"][:, :, t0:t0 + 1024].rearrange("k p t -> p k t"), writes=[xT])
            oT = S.sbp("oT", [128, 16, 1024], BF16)
            S.dma("sp", oT[:, 0:8, :], R["oTA"][:, :, t0:t0 + 1024].rearrange("k p t -> p k t"), writes=[oT])
            S.dma("sp", oT[:, 8:12, :], R["oTB"][:, :, t0:t0 + 1024].rearrange("k p t -> p k t"), writes=[oT])
            S.dma("sp", oT[:, 12:16, :], R["oTC"][:, :, t0:t0 + 1024].rearrange("k p t -> p k t"), writes=[oT])
            wgs = Ring([S.sbp("wg", [128, KC, 3, 128], BF16) for _ in range(2)])
            wbs = Ring([S.sbp("wb", [128, 16, 128], BF16) for _ in range(2)])
            sgs = Ring([S.sbp("sgb", [128, 512], BF16) for _ in range(3)])
            tmps = Ring([S.sbp("tmpm", [128, 512], F32) for _ in range(3)])
            maccs = Ring([S.sbp("macc", [128, 512], F32) for _ in range(2)])
            kranges = ((0, 8), (8, 12), (12, 16))
            def load_mw(dc):
                wgt = wgs.next()
                for gi in range(3):
                    c0 = OFF_MG + gi * 2048 + dc * 128
                    S.dma("pool", wgt[:, :, gi, :], I["w_in"][l, :, c0:c0 + 128].rearrange("(kc p) c -> p kc c", p=128),
                          writes=[wgt])
                wbt = wbs.next()
                S.dma("pool", wbt[:, 0:8, :], I["w_br_a"][l, :, dc * 128:(dc + 1) * 128].rearrange("(k p) c -> p k c", p=128),
                      writes=[wbt])
                S.dma("pool", wbt[:, 8:12, :], I["w_br_b"][l, :, dc * 128:(dc + 1) * 128].rearrange("(k p) c -> p k c", p=128),
                      writes=[wbt])
                S.dma("pool", wbt[:, 12:16, :], I["w_br_c"][l, :, dc * 128:(dc + 1) * 128].rearrange("(k p) c -> p k c", p=128),
                      writes=[wbt])
                return wgt, wbt

            nxt = load_mw(0)
            for dc in range(16):
                wgt, wbt = nxt
                if dc + 1 < 16:
                    nxt = load_mw(dc + 1)
                if dc == 1:
                    load_wo()
                for tb in range(2):
                    macc = maccs.next()
                    for gi in range(3):
                        psg = C.psS.next()
                        for kc in range(KC):
                            S.mm(psg[:], wgt[:, kc, gi, :], xT[:, kc, tb * 512:(tb + 1) * 512],
                                 start=(kc == 0), stop=(kc == KC - 1), reads=[wgt, xT], writes=[psg])
                        sgb = sgs.next()
                        S.op("act", lambda: nc.scalar.activation(sgb[:], psg[:], AF.Sigmoid), reads=[psg], writes=[sgb])
                        psb = C.psX.next()
                        k0, k1 = kranges[gi]
                        for k in range(k0, k1):
                            S.mm(psb[:], wbt[:, k, :], oT[:, k, tb * 512:(tb + 1) * 512],
                                 start=(k == k0), stop=(k == k1 - 1), reads=[wbt, oT], writes=[psb])
                        if gi == 0:
                            S.op("dve", lambda: nc.vector.tensor_tensor(out=macc[:], in0=psb[:], in1=sgb[:], op=ALU.mult),
                                 reads=[psb, sgb], writes=[macc])
                        else:
                            tmp = tmps.next()
                            S.op("dve", lambda: nc.vector.tensor_tensor(out=tmp[:], in0=psb[:], in1=sgb[:], op=ALU.mult),
                                 reads=[psb, sgb], writes=[tmp])
                            dst = macc[:] if gi == 1 else mT[:, dc, tb * 512:(tb + 1) * 512]
                            S.op("pool", lambda: nc.gpsimd.tensor_tensor(out=dst, in0=macc[:], in1=tmp[:], op=ALU.add),
                                 reads=[macc, tmp], writes=[macc] if gi == 1 else [mT])
        if "mg1" in C.dbg:
            return
        with S.phase():
            lng = S.sbp("lng", [128, D], F32)
            lnb = S.sbp("lnb", [128, D], F32)
            S.dma("sp", lng[:], I["ln1_g"][l].partition_broadcast(128), writes=[lng])
            S.dma("sp", lnb[:], I["ln1_b"][l].partition_broadcast(128), writes=[lnb])
            wr = S.sbp("wr", [128, KC, 36], F32)
            S.dma("sp", wr[:], I["w_rt"][l].rearrange("(p kc) c -> p kc c", kc=KC), writes=[wr])
            brt = S.sbp("brt", [128, 36], F32)
            S.dma("sp", brt[:], I["b_rt"][l].partition_broadcast(128), writes=[brt])
            xts = Ring([S.sbp("xt", [128, D], F32) for _ in range(2)])
            ys = Ring([S.sbp("y", [128, D], F32) for _ in range(2)])
            xns = Ring([S.sbp("xn", [128, D], F32) for _ in range(1)])
            x1s = Ring([S.sbp("x1t", [128, D], F32) for _ in range(2)])
            sts = Ring([S.sbp("st", [128, 4, 6], F32) for _ in range(2)])
            mvs = Ring([S.sbp("mv", [128, 8], F32) for _ in range(2)])
            x1Tb = Ring([S.sbp("x1Tb", [128, KC, 128], BF16) for _ in range(1)])
            x1Tf = Ring([S.sbp("x1Tf", [128, KC, 128], F32) for _ in range(2)])
            rts = Ring([S.sbp("rt", [128, 128], F32) for _ in range(3)])
            cTs = Ring([S.sbp("cTst", [32, 128], BF16) for _ in range(2)])
            x1_of = {}

            def stage_a(ti):
                i = blk * 8 + ti
                xt = xts.next()
                S.dma("sp", xt[:], x_in[i * 128:(i + 1) * 128, :], writes=[xt])
                y = ys.next()
                for db in range(4):
                    ps = C.psV[db]
                    for kc in range(KC):
                        S.mm(ps[:], mT[:, kc, ti * 128:(ti + 1) * 128], wo[:, kc, db * 512:(db + 1) * 512],
                             start=(kc == 0), stop=(kc == KC - 1), reads=[mT, wo], writes=[ps])
                    S.op("dve", lambda: nc.vector.scalar_tensor_tensor(
                        out=y[:, db * 512:(db + 1) * 512], in0=xt[:, db * 512:(db + 1) * 512], scalar=ALPHA, in1=ps[:],
                        op0=ALU.mult, op1=ALU.add), reads=[xt, ps], writes=[y])
                x1t = x1s.next()
                _layernorm_tile(C, y, lng, lnb, xns.next(), x1t, sts.next(), mvs.next())
                S.dma("sp", R["x1"][i * 128:(i + 1) * 128, :], x1t[:], reads=[x1t])
                x1_of[ti] = x1t

            xf_of, rt_of = {}, {}

            def stage_b1(ti):
                i = blk * 8 + ti
                x1t = x1_of.pop(ti)
                xb_ = x1Tb.next()
                xf_ = x1Tf.next()
                for q in range(4):
                    ps = C.psX.next()
                    for j in range(4):
                        kc = q * 4 + j
                        S.tr(ps[:, j * 128:(j + 1) * 128], x1t[:, kc:D:KC], C.ident[:],
                             reads=[x1t, C.ident], writes=[ps])
                    S.copy("act", xf_[:, q * 4:(q + 1) * 4, :], ps[:].rearrange("p (a b) -> p a b", a=4),
                           reads=[ps], writes=[xf_])
                    S.copy("dve", xb_[:, q * 4:(q + 1) * 4, :], xf_[:, q * 4:(q + 1) * 4, :],
                           reads=[xf_], writes=[xb_])
                if "mg5" not in C.dbg:
                    S.dma("sp", R["x1T"][:, :, i * 128:(i + 1) * 128].rearrange("k p t -> p k t"), xb_[:], reads=[xb_])
                xf_of[ti] = xf_

            def stage_b2(ti):
                i = blk * 8 + ti
                xf_ = xf_of.pop(ti)
                psr = C.psS.next()
                for kc in range(KC):
                    S.mm(psr[:, 0:36], xf_[:, kc, :], wr[:, kc, :], start=(kc == 0), stop=(kc == KC - 1),
                         reads=[xf_, wr], writes=[psr])
                rt = rts.next()
                LG, GM, GN, EM, T8, SL, SC = 0, 36, 40, 44, 76, 84, 116
                dv = lambda f, rd, wr_: S.op("dve", f, reads=rd, writes=wr_)
                dv(lambda: nc.vector.tensor_tensor(out=rt[:, LG:LG + 36], in0=psr[:, 0:36], in1=brt[:], op=ALU.add),
                   [psr, brt], [rt])
                dv(lambda: nc.vector.tensor_reduce(out=rt[:, SC:SC + 1], in_=rt[:, LG:LG + 4], axis=AX.X, op=ALU.max),
                   [rt], [rt])
                dv(lambda: nc.vector.tensor_scalar(out=rt[:, SC + 1:SC + 2], in0=rt[:, SC:SC + 1], scalar1=-1.0,
                                                   scalar2=None, op0=ALU.mult), [rt], [rt])
                S.op("act", lambda: nc.scalar.activation(rt[:, SC + 8:SC + 12], rt[:, LG:LG + 4], AF.Exp,
                                                         bias=rt[:, SC + 1:SC + 2], scale=1.0), reads=[rt], writes=[rt])
                dv(lambda: nc.vector.tensor_reduce(out=rt[:, SC + 2:SC + 3], in_=rt[:, SC + 8:SC + 12], axis=AX.X,
                                                   op=ALU.add), [rt], [rt])
                dv(lambda: nc.vector.reciprocal(rt[:, SC + 3:SC + 4], rt[:, SC + 2:SC + 3]), [rt], [rt])
                dv(lambda: nc.vector.tensor_scalar(out=rt[:, GM:GM + 4], in0=rt[:, LG:LG + 4], scalar1=rt[:, SC:SC + 1],
                                                   scalar2=None, op0=ALU.is_ge), [rt], [rt])
                dv(lambda: nc.vector.tensor_scalar(out=rt[:, GN:GN + 4], in0=rt[:, GM:GM + 4], scalar1=-1.0, scalar2=BIG,
                                                   op0=ALU.add, op1=ALU.mult), [rt], [rt])
                for g in range(4):
                    dv(lambda: nc.vector.tensor_scalar(
                        out=rt[:, EM + 8 * g:EM + 8 * g + 8], in0=rt[:, LG + 4 + 8 * g:LG + 12 + 8 * g],
                        scalar1=rt[:, GM + g:GM + g + 1], scalar2=rt[:, GN + g:GN + g + 1],
                        op0=ALU.mult, op1=ALU.add), [rt], [rt])
                dv(lambda: nc.vector.max(out=rt[:, T8:T8 + 8], in_=rt[:, EM:EM + 32]), [rt], [rt])
                dv(lambda: nc.vector.tensor_scalar(out=rt[:, SL:SL + 32], in0=rt[:, EM:EM + 32],
                                                   scalar1=rt[:, T8 + 1:T8 + 2], scalar2=None, op0=ALU.is_ge), [rt], [rt])
                dv(lambda: nc.vector.tensor_scalar(out=rt[:, SC + 4:SC + 5], in0=rt[:, T8:T8 + 1], scalar1=-1.0,
                                                   scalar2=None, op0=ALU.mult), [rt], [rt])
                dv(lambda: nc.vector.tensor_scalar(out=rt[:, EM:EM + 32], in0=rt[:, EM:EM + 32], scalar1=-1.0e4,
                                                   scalar2=None, op0=ALU.max), [rt], [rt])
                S.op("act", lambda: nc.scalar.activation(rt[:, EM:EM + 32], rt[:, EM:EM + 32], AF.Exp,
                                                         bias=rt[:, SC + 4:SC + 5], scale=1.0), reads=[rt], writes=[rt])
                S.op("act", lambda: nc.scalar.activation(rt[:, SC + 5:SC + 6], rt[:, T8 + 1:T8 + 2], AF.Exp,
                                                         bias=rt[:, SC + 4:SC + 5], scale=1.0), reads=[rt], writes=[rt])
                dv(lambda: nc.vector.tensor_scalar(out=rt[:, SC + 5:SC + 6], in0=rt[:, SC + 5:SC + 6], scalar1=1.0,
                                                   scalar2=None, op0=ALU.add), [rt], [rt])
                dv(lambda: nc.vector.reciprocal(rt[:, SC + 6:SC + 7], rt[:, SC + 5:SC + 6]), [rt], [rt])
                dv(lambda: nc.vector.tensor_tensor(out=rt[:, SC + 7:SC + 8], in0=rt[:, SC + 6:SC + 7],
                                                   in1=rt[:, SC + 3:SC + 4], op=ALU.mult), [rt], [rt])
                dv(lambda: nc.vector.tensor_tensor(out=rt[:, SL:SL + 32], in0=rt[:, SL:SL + 32], in1=rt[:, EM:EM + 32],
                                                   op=ALU.mult), [rt], [rt])
                dv(lambda: nc.vector.tensor_scalar(out=rt[:, SL:SL + 32], in0=rt[:, SL:SL + 32],
                                                   scalar1=rt[:, SC + 7:SC + 8], scalar2=None, op0=ALU.mult), [rt], [rt])
                rt_of[ti] = rt

            def stage_b3(ti):
                i = blk * 8 + ti
                rt = rt_of.pop(ti)
                LG, GM, GN, EM, T8, SL, SC = 0, 36, 40, 44, 76, 84, 116
                pst = C.psX.next()
                S.tr(pst[0:32, 0:128], rt[:, SL:SL + 32], C.ident[:], reads=[rt, C.ident], writes=[pst])
                cst = cTs.next()
                S.copy("act", cst[:], pst[0:32, 0:128], reads=[pst], writes=[cst])
                S.dma("sp", R["cT"][:, i * 128:(i + 1) * 128], cst[:], reads=[cst])

            for k in range(-2, 9):
                if 0 <= k + 2 < 8:
                    stage_a(k + 2)
                if 0 <= k + 1 < 8:
                    stage_b1(k + 1)
                if 0 <= k < 8:
                    stage_b2(k)
                if 0 <= k - 1 < 8:
                    stage_b3(k - 1)


def phase_moe(C, blk, x_out):
    nc, S, I, R, T = C.nc, C.S, C.I, C.R, C.T
    l = C.l
    ev = _evac_engines()
    t0 = blk * 1024
    with S.phase():
      yacc = S.sbp("yacc", [128, 8, D], F32)
      yparts = [[S.tok("yp") for _ in range(4)] for _ in range(8)]
      with S.phase():
        x1T = S.sbp("x1T", [128, KC, 1024], BF16)
        S.dma("sp", x1T[:], R["x1T"][:, :, t0:t0 + 1024].rearrange("k p t -> p k t"), writes=[x1T])
        cT = S.sbp("cT", [128, 1024], BF16)
        S.op("pool", lambda: nc.gpsimd.memset(cT[:], 0.0), writes=[cT])
        S.dma("sp", cT[0:32], R["cT"][:, t0:t0 + 1024], writes=[cT])
        oh = S.sbp("oh", [128, 32, 128], BF16)
        S.op("pool", lambda: nc.gpsimd.memset(oh[:], 0.0), writes=[oh])
        S.dma("pool", oh[0:32], T["onehot"].rearrange("p (e c) -> p e c", c=128), writes=[oh])
        wgus = Ring([S.sbp("wgu", [128, 2, KC, 256], BF16) for _ in range(2)])
        wds = Ring([S.sbp("wd", [128, 2, 2, D], BF16) for _ in range(2)])
        aTs = Ring([S.sbp("aT", [128, 2, 2, 1024], BF16) for _ in range(2)])
        cbs = Ring([S.sbp("cb", [128, 1024], BF16) for _ in range(2)])
        sgs = Ring([S.sbp("sg", [128, 512], BF16) for _ in range(3)])
        t1s = Ring([S.sbp("t1", [128, 512], BF16) for _ in range(3)])
        psH = Ring([C.psS.bufs[0], C.psS.bufs[1], C.psX.bufs[0], C.psX.bufs[1]])
        psC = Ring([C.psV[2], C.psV[3]])
        psY = Ring([C.psV[0], C.psV[1]])
        def load_gu(e):
            w = wgus.next()
            S.dma("pool", w[:, 0].rearrange("p (a k) f -> p a (k f)", a=2),
                  I["w_gate"][l, e].rearrange("(p a k) f -> p a (k f)", a=2, k=KC // 2), writes=[w])
            S.dma("pool", w[:, 1].rearrange("p (a k) f -> p a (k f)", a=2),
                  I["w_up"][l, e].rearrange("(p a k) f -> p a (k f)", a=2, k=KC // 2), writes=[w])
            return w

        def load_d(ep):
            wdt = wds.next()
            for j in range(2):
                S.dma("pool", wdt[:, j], I["w_down"][l, 2 * ep + j].rearrange("(p fc) d -> p fc d", fc=2), writes=[wdt])
            return wdt

        nxt_w = load_gu(0)
        nxt_d = load_d(0)
        for ep in range(16):
            wdt = nxt_d
            a = aTs.next()
            for j in range(2):
                e = 2 * ep + j
                w = nxt_w
                if e + 1 < 32:
                    nxt_w = load_gu(e + 1)
                if j == 0 and ep + 1 < 16:
                    nxt_d = load_d(ep + 1)
                cbt = cbs.next()
                for tb in range(2):
                    psc = psC.next()
                    S.mm(psc[:], oh[:, e, :], cT[:, tb * 512:(tb + 1) * 512], reads=[oh, cT], writes=[psc])
                    S.copy("act", cbt[:, tb * 512:(tb + 1) * 512], psc[:], reads=[psc], writes=[cbt])
                for fc in range(2):
                    for tb in range(2):
                        psg = psH.next()
                        for kc in range(KC):
                            S.mm(psg[:], w[:, 0, kc, fc:256:2], x1T[:, kc, tb * 512:(tb + 1) * 512],
                                 start=(kc == 0), stop=(kc == KC - 1), reads=[w, x1T], writes=[psg])
                        psu = psH.next()
                        for kc in range(KC):
                            S.mm(psu[:], w[:, 1, kc, fc:256:2], x1T[:, kc, tb * 512:(tb + 1) * 512],
                                 start=(kc == 0), stop=(kc == KC - 1), reads=[w, x1T], writes=[psu])
                        sg = sgs.next()
                        S.op("act", lambda: nc.scalar.activation(sg[:], psg[:], AF.Silu), reads=[psg], writes=[sg])
                        t1 = t1s.next()
                        S.op("dve", lambda: nc.vector.tensor_tensor(out=t1[:], in0=psu[:], in1=sg[:], op=ALU.mult),
                             reads=[psu, sg], writes=[t1])
                        S.op("pool", lambda: nc.gpsimd.tensor_tensor(
                            out=a[:, j, fc, tb * 512:(tb + 1) * 512], in0=t1[:], in1=cbt[:, tb * 512:(tb + 1) * 512],
                            op=ALU.mult), reads=[t1, cbt], writes=[a])
            for ti in range(8):
                for db in range(4):
                    psy = psY.next()
                    n = 0
                    for j in range(2):
                        for fc in range(2):
                            S.mm(psy[:], a[:, j, fc, ti * 128:(ti + 1) * 128], wdt[:, j, fc, db * 512:(db + 1) * 512],
                                 start=(n == 0), stop=(n == 3), reads=[a, wdt], writes=[psy])
                            n += 1
                    yp = yparts[ti][db]
                    ysl = yacc[:, ti, db * 512:(db + 1) * 512]
                    if ep == 0:
                        S.copy(next(ev), ysl, psy[:], reads=[psy], writes=[yp])
                    else:
                        S.op("dve", lambda: nc.vector.tensor_tensor(out=ysl, in0=psy[:], in1=ysl, op=ALU.add),
                             reads=[psy, yp], writes=[yp])
      with S.phase():
        lng = S.sbp("lng2", [128, D], F32)
        lnb = S.sbp("lnb2", [128, D], F32)
        S.dma("sp", lng[:], I["ln2_g"][l].partition_broadcast(128), writes=[lng])
        S.dma("sp", lnb[:], I["ln2_b"][l].partition_broadcast(128), writes=[lnb])
        xts = Ring([S.sbp("x1r", [128, D], F32) for _ in range(3)])
        outs = Ring([S.sbp("x2t", [128, D], F32) for _ in range(3)])
        sts2 = Ring([S.sbp("st2", [128, 4, 6], F32) for _ in range(3)])
        mvs2 = Ring([S.sbp("mv2", [128, 8], F32) for _ in range(3)])
        ln_state = {}

        def ln2_s1(ti):
            i = blk * 8 + ti
            xt = xts.next()
            S.dma("sp", xt[:], R["x1"][i * 128:(i + 1) * 128, :], writes=[xt])
            ytok = S.tok("ytile")
            for db in range(4):
                S.op("dve", lambda: nc.vector.scalar_tensor_tensor(
                    out=yacc[:, ti, db * 512:(db + 1) * 512], in0=xt[:, db * 512:(db + 1) * 512], scalar=ALPHA,
                    in1=yacc[:, ti, db * 512:(db + 1) * 512], op0=ALU.mult, op1=ALU.add),
                    reads=[xt, yparts[ti][db]], writes=[yparts[ti][db], ytok])
            yv = Buf("yv", None)
            yv.last_write, yv.reads = ytok.last_write, []
            yv.__class__ = type("YV", (Buf,), {"__getitem__": lambda self, idx, ti=ti: yacc[(slice(None), ti) + tuple(idx[1:])] if isinstance(idx, tuple) else yacc[:, ti]})
            mv = mvs2.next()
            _layernorm_tile(C, yv, lng, lnb, xt, None, sts2.next(), mv, part=1)
            ln_state[ti] = (yv, xt, mv)

        def ln2_s2(ti):
            i = blk * 8 + ti
            yv, xt, mv = ln_state.pop(ti)
            ot = outs.next()
            _ln_part2(C, yv, lng, lnb, xt, ot, mv)
            S.dma("sp", x_out[i * 128:(i + 1) * 128, :], ot[:], reads=[ot])

        ln2_s1(0)
        for ti in range(8):
            if ti + 1 < 8:
                ln2_s1(ti + 1)
            ln2_s2(ti)
```

```python
import numpy as np
import concourse.bass as bass
import concourse.mybir as mybir
from concourse.bass_utils import run_bass_kernel_spmd

F32 = mybir.dt.float32
BF16 = mybir.dt.bfloat16
AF = mybir.ActivationFunctionType
ALU = mybir.AluOpType
AX = mybir.AxisListType

SEM_LIMIT = 3000


class Buf:
    def __init__(self, name, t=None):
        self.name = name
        self.t = t
        self.last_write = None
        self.reads = []

    def ap(self):
        return self.t[:] if not hasattr(self.t, "ap") else self.t.ap()

    def __getitem__(self, idx):
        return self.t[idx]


class _Eng:
    def __init__(self, name, handle):
        self.name = name
        self.h = handle
        self.sem = None
        self.count = 0
        self.seen = {}


class Sched:
    def __init__(self, nc, n_dma_sems=32):
        self.nc = nc
        self.engs = {
            "pe": _Eng("pe", nc.tensor),
            "act": _Eng("act", nc.scalar),
            "dve": _Eng("dve", nc.vector),
            "pool": _Eng("pool", nc.gpsimd),
            "sp": _Eng("sp", nc.sync),
        }
        self.sems = {}
        self._nsem = 0
        for e in self.engs.values():
            self._new_eng_sem(e)
        self.dma_sems = []
        for i in range(n_dma_sems):
            k = self._alloc_sem("dma%d" % i)
            self.dma_sems.append([k, 0])
        self.dma_rr = 0
        self.ninst = 0

    def _alloc_sem(self, name):
        self._nsem += 1
        key = "%s_%d" % (name, self._nsem)
        self.sems[key] = self.nc.alloc_semaphore(name=key)
        return key

    def _new_eng_sem(self, e):
        e.sem = self._alloc_sem("c_" + e.name)
        e.count = 0

    def sb(self, name, shape, dtype):
        return Buf(name, self.nc.alloc_sbuf_tensor(name, list(shape), dtype))

    def ps(self, name, shape, dtype=F32):
        return Buf(name, self.nc.alloc_psum_tensor(name, list(shape), dtype))

    def tok(self, name):
        return Buf(name, None)

    def _deps(self, reads, writes):
        need = {}
        for b in reads:
            if b.last_write is not None:
                k, v = b.last_write
                need[k] = max(need.get(k, 0), v)
        for b in writes:
            if b.last_write is not None:
                k, v = b.last_write
                need[k] = max(need.get(k, 0), v)
            for k, v in b.reads:
                need[k] = max(need.get(k, 0), v)
        return need

    def _emit_waits(self, e, need):
        for k, v in need.items():
            if e.seen.get(k, 0) < v:
                e.h.wait_ge(self.sems[k], v)
                e.seen[k] = v

    def _record(self, reads, writes, key, val):
        for b in reads:
            b.reads.append((key, val))
        for b in writes:
            b.last_write = (key, val)
            b.reads = []

    def op(self, eng, fn, reads=(), writes=()):
        e = self.engs[eng]
        if e.count >= SEM_LIMIT:
            self._new_eng_sem(e)
        need = self._deps(reads, writes)
        self._emit_waits(e, need)
        inst = fn()
        e.count += 1
        inst.then_inc(self.sems[e.sem], 1)
        if eng == "pe":
            e.seen[e.sem] = e.count
        self._record(reads, writes, e.sem, e.count)
        self.ninst += 1
        return inst

    def dma(self, queue, out, in_, reads=(), writes=(), **kw):
        e = self.engs[queue]
        slot = self.dma_sems[self.dma_rr]
        self.dma_rr = (self.dma_rr + 1) % len(self.dma_sems)
        key, cur = slot
        if cur + 16 > SEM_LIMIT:
            key = self._alloc_sem("dmax")
            slot[0] = key
            cur = 0
        need = self._deps(reads, writes)
        if cur > 0:
            need[key] = max(need.get(key, 0), cur)
        self._emit_waits(e, need)
        inst = e.h.dma_start(out=out, in_=in_, **kw)
        inst.then_inc(self.sems[key], 16)
        slot[1] = cur + 16
        self._record(reads, writes, key, cur + 16)
        self.ninst += 1
        return inst

    def finish(self):
        e = self.engs["sp"]
        need = {}
        for key, cur in self.dma_sems:
            if cur > 0:
                need[key] = cur
        for o in self.engs.values():
            if o is not e and o.count > 0:
                need[o.sem] = o.count
        self._emit_waits(e, need)


D = 2048
KC = 16
TOK = 2048
NT = TOK // 128
DEPTH = 2
ALPHA = (2.0 * DEPTH) ** 0.25
LN_EPS = 1e-5
D_IN = 11032
OFF_QA, OFF_KVA, OFF_GA, OFF_QB, OFF_KB, OFF_VB, OFF_QC, OFF_KC_, OFF_VC, OFF_MG = (
    0, 1024, 2560, 2584, 3096, 3224, 3352, 3864, 4376, 4888)
BIG = 1.0e30
NEGB = 30000.0
SC_A = 128.0 ** -0.5
SC_B = 64.0 ** -0.5


class Ring:
    def __init__(self, bufs):
        self.bufs = bufs
        self.i = 0

    def next(self):
        b = self.bufs[self.i % len(self.bufs)]
        self.i += 1
        return b


class Phase:
    def __init__(self, S):
        self.S = S

    def __enter__(self):
        from contextlib import ExitStack
        self.stack = ExitStack()
        self.prev = getattr(self.S, "stack", None)
        self.S.stack = self.stack
        return self

    def __exit__(self, *a):
        self.S.barrier()
        self.stack.close()
        self.S.stack = self.prev
        return False


def _phase(self):
    return Phase(self)


def _sbp(self, name, shape, dtype):
    self._nbuf = getattr(self, "_nbuf", 0) + 1
    nm = "%s_%d" % (name, self._nbuf)
    t = self.stack.enter_context(self.nc.sbuf_tensor(nm, list(shape), dtype))
    return Buf(nm, t)


def _barrier(self):
    engs = list(self.engs.values())
    need_all = {}
    for key, cur in self.dma_sems:
        if cur > 0:
            need_all[key] = cur
    for o in engs:
        if o.count > 0:
            need_all[o.sem] = o.count
    for e in engs:
        self._emit_waits(e, dict(need_all))


def _mm(self, out, lhsT, rhs, start=True, stop=True, reads=(), writes=()):
    nc = self.nc
    return self.op("pe", lambda: nc.tensor.matmul(out, lhsT, rhs, start=start, stop=stop),
                   reads=reads, writes=writes)


def _tr(self, out, in_, ident, reads=(), writes=()):
    nc = self.nc
    return self.op("pe", lambda: nc.tensor.transpose(out, in_, ident), reads=reads, writes=writes)


def _copy(self, eng, out, in_, reads=(), writes=()):
    nc = self.nc
    if eng == "act":
        f = lambda: nc.scalar.copy(out, in_)
    elif eng == "dve":
        f = lambda: nc.vector.tensor_copy(out, in_)
    else:
        f = lambda: nc.gpsimd.tensor_copy(out, in_)
    return self.op(eng, f, reads=reads, writes=writes)


Sched.phase = _phase
Sched.sbp = _sbp
Sched.barrier = _barrier
Sched.mm = _mm
Sched.tr = _tr
Sched.copy = _copy


class Ctx:
    pass


def build_program(n_layers=DEPTH, debug=None):
    nc = bass.Bass("TRN2", target_bir_lowering=False)
    S = Sched(nc)
    C = Ctx()
    C.nc, C.S = nc, S
    dbg = debug or ()
    C.dbg = dbg

    def din(name, shape, dt=F32):
        return nc.dram_tensor(name, list(shape), dt, kind="ExternalInput").ap()

    def dscr(name, shape, dt):
        if name in dbg:
            return nc.dram_tensor(name, list(shape), dt, kind="ExternalOutput").ap()
        return nc.dram_tensor(name, list(shape), dt).ap()

    shapes = {
        "x": [TOK, D], "w_in": [DEPTH, D, D_IN], "nsa_cmp_pos": [DEPTH, 64, 128],
        "nsa_cmp_w": [DEPTH, 2, 32, 128, 128], "sink_b": [DEPTH, 8], "w_br_a": [DEPTH, 1024, D],
        "w_br_b": [DEPTH, 512, D], "w_br_c": [DEPTH, 512, D], "w_out": [DEPTH, D, D],
        "ln1_g": [DEPTH, D], "ln1_b": [DEPTH, D], "ln2_g": [DEPTH, D], "ln2_b": [DEPTH, D],
        "w_rt": [DEPTH, D, 36], "b_rt": [DEPTH, 36], "w_gate": [DEPTH, 32, D, 256],
        "w_up": [DEPTH, 32, D, 256], "w_down": [DEPTH, 32, 256, D],
        "t_ident": [128, 128], "t_maskA": [128, 2 * 17 * 4 * 128], "t_maskB": [128, 2 * 2 * 4 * 128],
        "t_maskC": [128, 16 * 4 * 128], "t_gcmp": [128, 8 * 248], "t_inter": [128, 33],
        "t_selmul": [128, NT * 32], "t_seladd": [128, NT * 32], "t_cmul": [128, NT * 32],
        "t_cadd": [128, NT * 32], "t_cown": [128, NT * 32], "t_eselA": [32, NT * 128],
        "t_eselC": [8, NT * 128], "t_onehot": [32, 32 * 128],
    }

    class Lazy(dict):
        def __init__(self, prefix=""):
            super().__init__()
            self.prefix = prefix

        def __missing__(self, key):
            v = din(self.prefix + key, shapes[self.prefix + key])
            self[key] = v
            return v

    I = Lazy()
    T = Lazy("t_")
    out = nc.dram_tensor("out", [TOK, D], F32, kind="ExternalOutput").ap()

    R = {}
    R["xT"] = dscr("s_xT", [KC, 128, TOK], BF16)
    R["qTA"] = dscr("s_qTA", [8, 128, TOK], BF16)
    R["kTA"] = dscr("s_kTA", [3, 2, 128, TOK], BF16)
    R["vTcmp"] = dscr("s_vTcmp", [2, 128, TOK], BF16)
    R["vA"] = dscr("s_vA", [2, TOK, 256], BF16)
    R["gateA"] = dscr("s_gateA", [TOK, 24], F32)
    R["qTB"] = dscr("s_qTB", [8, 64, TOK], BF16)
    R["kTB"] = dscr("s_kTB", [2, 64, TOK], BF16)
    R["vB"] = dscr("s_vB", [TOK, 128], BF16)
    R["qTC"] = dscr("s_qTC", [4, 128, TOK], BF16)
    R["kTC"] = dscr("s_kTC", [4, 128, TOK], BF16)
    R["vC"] = dscr("s_vC", [TOK, 512], BF16)
    R["oTA"] = dscr("s_oTA", [8, 128, TOK], BF16)
    R["oTB"] = dscr("s_oTB", [4, 128, TOK], BF16)
    R["oTC"] = dscr("s_oTC", [4, 128, TOK], BF16)
    R["x1"] = dscr("s_x1", [TOK, D], F32)
    R["x1T"] = dscr("s_x1T", [KC, 128, TOK], BF16)
    R["cT"] = dscr("s_cT", [32, TOK], BF16)
    R["xmid"] = dscr("s_xmid", [TOK, D], F32)
    C.I, C.T, C.R = I, T, R

    C.psS = Ring([S.ps("psS%d" % i, [128, 512]) for i in range(2)])
    C.psV = [S.ps("psV%d" % i, [128, 512]) for i in range(4)]
    C.psX = Ring([S.ps("psX%d" % i, [128, 512]) for i in range(2)])
    C.ident = S.sb("ident", [128, 128], F32)
    C.identb = S.sb("identb", [128, 128], BF16)
    S.dma("sp", C.ident[:], T["ident"], writes=[C.ident])
    C.used = lambda: [k for k in I.keys()] + ["t_" + k for k in T.keys()]
    S.op("dve", lambda: nc.vector.tensor_copy(C.identb[:], C.ident[:]), reads=[C.ident], writes=[C.identb])

    for l in range(n_layers):
        x_in = I["x"] if l == 0 else R["xmid"]
        x_out = out if l == n_layers - 1 else R["xmid"]
        C.l = l
        phase_inproj(C, x_in)
        if "stop_inproj" in dbg:
            break
        phase_mixA(C)
        phase_mixB(C)
        phase_mixC(C)
        if "stop_mix" in dbg:
            break
        for blk in range(2):
            phase_merge(C, x_in, blk)
        if "stop_merge" in dbg:
            break
        for blk in range(2):
            phase_moe(C, blk, x_out)
    S.finish()
    return nc, C.used()


def _evac_engines():
    while True:
        yield "act"
        yield "dve"


def phase_inproj(C, x_in):
    nc, S, I, R = C.nc, C.S, C.I, C.R
    l = C.l
    ev = _evac_engines()
    with S.phase():
        xT = S.sbp("xT", [128, KC, TOK], BF16)
        xparts = [S.tok("xTp%d" % i) for i in range(NT)]
        xin = Ring([S.sbp("xin", [128, D], F32) for _ in range(2)])
        for i in range(NT):
            xb = xin.next()
            S.dma("sp", xb[:], x_in[i * 128:(i + 1) * 128, :], writes=[xb])
            for q in range(4):
                ps = C.psX.next()
                for j in range(4):
                    kc = q * 4 + j
                    S.tr(ps[:, j * 128:(j + 1) * 128], xb[:, kc * 128:(kc + 1) * 128], C.ident[:],
                         reads=[xb, C.ident], writes=[ps])
                S.copy(next(ev), xT[:, q * 4:(q + 1) * 4, i * 128:(i + 1) * 128],
                       ps[:].rearrange("p (a b) -> p a b", a=4), reads=[ps], writes=[xparts[i]])
        for kc in range(KC):
            S.dma("sp", R["xT"][kc], xT[:, kc, :], reads=xparts)

        wbuf = Ring([S.sbp("wbuf", [128, KC, 512], BF16) for _ in range(2)])
        stg = Ring([S.sbp("stg", [128, TOK], BF16) for _ in range(2)])
        stgt = Ring([S.sbp("stgt", [128, 512], BF16) for _ in range(3)])
        stgf = Ring([S.sbp("stgf", [128, 24], F32) for _ in range(2)])
        w_in = I["w_in"]

        def load_w(col0, width):
            wt = wbuf.next()
            src = w_in[l, :, col0:col0 + width].rearrange("(kc p) c -> p kc c", p=128)
            S.dma("pool", wt[:, :, 0:width], src, writes=[wt])
            return wt

        def fm_job(col0, width, sub, dsts):
            wt = load_w(col0, width)
            for j in range(width // sub):
                st = stg.next()
                for tb in range(4):
                    ps = C.psX.next()
                    for kc in range(KC):
                        S.mm(ps[0:sub, :], wt[:, kc, j * sub:(j + 1) * sub], xT[:, kc, tb * 512:(tb + 1) * 512],
                             start=(kc == 0), stop=(kc == KC - 1),
                             reads=[wt] + xparts[tb * 4:(tb + 1) * 4], writes=[ps])
                    S.copy(next(ev), st[0:sub, tb * 512:(tb + 1) * 512], ps[0:sub, :], reads=[ps], writes=[st])
                S.dma("sp", dsts[j], st[0:sub, :], reads=[st])

        def tm_job(col0, width, dst, sigmoid=False):
            wt = load_w(col0, width)
            for i in range(NT):
                ps = C.psX.next()
                for kc in range(KC):
                    S.mm(ps[:, 0:width], xT[:, kc, i * 128:(i + 1) * 128], wt[:, kc, 0:width],
                         start=(kc == 0), stop=(kc == KC - 1), reads=[wt, xparts[i]], writes=[ps])
                if sigmoid:
                    st = stgf.next()
                    S.op("act", lambda: nc.scalar.activation(st[:, 0:width], ps[:, 0:width], AF.Sigmoid),
                         reads=[ps], writes=[st])
                else:
                    st = stgt.next()
                    S.copy(next(ev), st[:, 0:width], ps[:, 0:width], reads=[ps], writes=[st])
                S.dma("sp", dst[i * 128:(i + 1) * 128, :], st[:, 0:width], reads=[st])

        fm_job(OFF_QA, 512, 128, [R["qTA"][h] for h in range(4)])
        fm_job(OFF_QA + 512, 512, 128, [R["qTA"][h] for h in range(4, 8)])
        fm_job(OFF_KVA, 512, 128, [R["kTA"][0, 0], R["kTA"][0, 1], R["vTcmp"][0], R["vTcmp"][1]])
        fm_job(OFF_KVA + 512, 256, 128, [R["kTA"][1, 0], R["kTA"][1, 1]])
        tm_job(OFF_KVA + 768, 256, R["vA"][0])
        fm_job(OFF_KVA + 1024, 256, 128, [R["kTA"][2, 0], R["kTA"][2, 1]])
        tm_job(OFF_KVA + 1280, 256, R["vA"][1])
        tm_job(OFF_GA, 24, R["gateA"], sigmoid=True)
        fm_job(OFF_QB, 512, 64, [R["qTB"][h] for h in range(8)])
        fm_job(OFF_KB, 128, 64, [R["kTB"][g] for g in range(2)])
        tm_job(OFF_VB, 128, R["vB"])
        fm_job(OFF_QC, 512, 128, [R["qTC"][h] for h in range(4)])
        fm_job(OFF_KC_, 512, 128, [R["kTC"][h] for h in range(4)])
        tm_job(OFF_VC, 512, R["vC"])


def _st_attention(C, kts, sel_emit, qk_emit, mask_of, v_of, dv, scale, ebuf, pbuf, par):
    nc, S = C.nc, C.S
    n = len(kts)
    pss = {}

    if not hasattr(C, "psS4"):
        C.psS4 = Ring([C.psS.bufs[0], C.psS.bufs[1], C.psX.bufs[0], C.psX.bufs[1]])
    LOOK = 3

    def emit_scores(idx):
        ps = C.psS4.next()
        first = True
        if sel_emit is not None:
            sel_emit(ps, kts[idx])
            first = False
        qk_emit(ps, kts[idx], first)
        pss[idx] = ps

    for j in range(min(LOOK, n)):
        emit_scores(j)
    for idx, kt in enumerate(kts):
        if idx + LOOK < n:
            emit_scores(idx + LOOK)
        ps = pss.pop(idx)
        E = ebuf.next()
        S.op("act", lambda: nc.scalar.activation(E[:], ps[:], AF.Exp, scale=scale), reads=[ps], writes=[E])
        P = pbuf.next()
        mk, mtok = mask_of(kt)
        eng = "dve"
        par[0] += 1
        h = nc.gpsimd if eng == "pool" else nc.vector
        S.op(eng, lambda: h.tensor_tensor(out=P[:].rearrange("p (a b) -> p a b", a=4),
                                          in0=E[:].rearrange("p (a b) -> p a b", a=4), in1=mk, op=ALU.mult),
             reads=[E, mtok], writes=[P])
        for hh in range(4):
            va, vtok = v_of(kt, hh)
            S.mm(C.psV[hh][:, 0:dv + 1], P[:, hh * 128:(hh + 1) * 128], va,
                 start=(idx == 0), stop=(idx == n - 1), reads=[P, vtok], writes=[C.psV[hh]])
    _flush_pending(C)


def _flush_pending(C):
    p = getattr(C, "pending", None)
    C.pending = None
    if p is not None:
        p()


def _post_bundle(C, dv, dsts, gates, first, small, extras=None, defer=False):
    nc, S = C.nc, C.S
    ncol = C.pvcols
    pvs, dens = [], []
    for hh in range(4):
        pvp = C.psV[hh]
        pv = C.pvbuf.next()
        S.op("act", lambda: nc.scalar.copy(pv[:, 0:ncol], pvp[:, 0:ncol]), reads=[pvp], writes=[pv])
        pvs.append(pv)
        dens.append(small.next())
    if defer:
        _flush_pending(C)
        C.pending = lambda: _post_chain(C, dv, dsts, gates, first, extras, pvs, dens)
        return dens, pvs
    _post_chain(C, dv, dsts, gates, first, extras, pvs, dens)
    return dens, pvs


def _post_chain(C, dv, dsts, gates, first, extras, pvs, dens):
    nc, S = C.nc, C.S
    for hh in range(4):
        pv, den = pvs[hh], dens[hh]
        if extras is not None:
            ex, extok = extras[hh]
            S.op("dve", lambda: nc.vector.tensor_tensor(out=den[:, 0:1], in0=pv[:, dv:dv + 1], in1=ex, op=ALU.add),
                 reads=[pv, extok], writes=[den])
        else:
            S.op("dve", lambda: nc.vector.tensor_scalar(out=den[:, 0:1], in0=pv[:, dv:dv + 1], scalar1=1e-30,
                                                        scalar2=None, op0=ALU.max), reads=[pv], writes=[den])
    for hh in range(4):
        den = dens[hh]
        S.op("dve", lambda: nc.vector.reciprocal(den[:, 1:2], den[:, 0:1]), reads=[den], writes=[den])
    ws = []
    for hh in range(4):
        den = dens[hh]
        if gates is not None:
            gap, gtok = gates[hh]
            S.op("dve", lambda: nc.vector.tensor_tensor(out=den[:, 2:3], in0=den[:, 1:2], in1=gap, op=ALU.mult),
                 reads=[den, gtok], writes=[den])
            ws.append(den[:, 2:3])
        else:
            ws.append(den[:, 1:2])
    for hh in range(4):
        pv, den, w = pvs[hh], dens[hh], ws[hh]
        dap, dtok = dsts[hh]
        if first:
            S.op("dve", lambda: nc.vector.tensor_scalar(out=dap, in0=pv[:, 0:dv], scalar1=w, scalar2=None,
                                                        op0=ALU.mult), reads=[pv, den], writes=[dtok])
        else:
            S.op("dve", lambda: nc.vector.scalar_tensor_tensor(out=dap, in0=pv[:, 0:dv], scalar=w, in1=dap,
                                                               op0=ALU.mult, op1=ALU.add),
                 reads=[pv, den, dtok], writes=[dtok])
    return dens, pvs


def _finish_tile(C, oacc, nchunk, ob, ost, dst, i, ev):
    nc, S = C.nc, C.S
    _flush_pending(C)
    S.op("act", lambda: nc.scalar.copy(ob[:, 0:nchunk * 128], oacc[:].rearrange("p a b -> p (a b)")),
         reads=[oacc], writes=[ob])
    ps = C.psX.next()
    psb = ps[:].bitcast(BF16)
    for c in range(nchunk):
        S.tr(psb[:, c * 128:(c + 1) * 128], ob[:, c * 128:(c + 1) * 128], C.identb[:],
             reads=[ob, C.identb], writes=[ps])
    S.copy(next(ev), ost[:, 0:nchunk * 128], psb[:, 0:nchunk * 128], reads=[ps], writes=[ost])
    S.dma("sp", dst[:, :, i * 128:(i + 1) * 128].rearrange("c p t -> p c t"),
          ost[:, 0:nchunk * 128].rearrange("p (c t) -> p c t", c=nchunk), reads=[ost])


def phase_mixA(C):
    nc, S, I, R, T = C.nc, C.S, C.I, C.R, C.T
    l = C.l
    ev = _evac_engines()
    par = [0]
    with S.phase():
        MA = S.sbp("MA", [128, 2, 17, 4, 128], BF16)
        for g in range(2):
            S.dma("pool", MA[:, g].rearrange("p a b c -> p (a b) c"),
                  T["maskA"][:, g * 8704:(g + 1) * 8704].rearrange("p (a c) -> p a c", c=128), writes=[MA])
        Gc = S.sbp("Gc", [128, 8, 248], BF16)
        S.dma("pool", Gc[:], T["gcmp"].rearrange("p (a c) -> p a c", c=248), writes=[Gc])
        kT = S.sbp("kT", [128, 2, 2, TOK], BF16)
        for b in range(2):
            for g in range(2):
                S.dma("sp", kT[:, b, g, :], R["kTA"][1 + b, g], writes=[kT])
        vaug = S.sbp("vaug", [128, 2, 2, NT, 129], BF16)
        S.op("pool", lambda: nc.gpsimd.memset(vaug[:], 1.0), writes=[vaug])
        for b in range(2):
            for g in range(2):
                S.dma("sp", vaug[:, b, g, :, 0:128],
                      R["vA"][b][:, g * 128:(g + 1) * 128].rearrange("(kt p) d -> p kt d", p=128), writes=[vaug])
        gt = S.sbp("gt", [128, NT, 24], F32)
        S.dma("sp", gt[:], R["gateA"].rearrange("(i p) c -> p i c", p=128), writes=[gt])
        selmul = S.sbp("selmul", [128, NT, 32], F32)
        seladd = S.sbp("seladd", [128, NT, 32], F32)
        S.dma("sp", selmul[:], T["selmul"].rearrange("p (i c) -> p i c", c=32), writes=[selmul])
        S.dma("sp", seladd[:], T["seladd"].rearrange("p (i c) -> p i c", c=32), writes=[seladd])
        esel = S.sbp("esel", [128, NT, 128], BF16)
        S.op("pool", lambda: nc.gpsimd.memset(esel[:], 0.0), writes=[esel])
        S.dma("pool", esel[0:32], T["eselA"].rearrange("p (i c) -> p i c", c=128), writes=[esel])

        kcin = S.sbp("kcin", [128, 2, TOK], BF16)
        vcin = S.sbp("vcin", [128, 2, TOK], BF16)
        for g in range(2):
            S.dma("sp", kcin[:, g, :], R["kTA"][0, g], writes=[kcin])
            S.dma("sp", vcin[:, g, :], R["vTcmp"][g], writes=[vcin])
        cw = S.sbp("cw", [128, 2, 32, 128], BF16)
        for k in range(2):
            S.dma("pool", cw[:, k], I["nsa_cmp_w"][l, k].rearrange("l d e -> d l e"), writes=[cw])
        posin = S.sbp("posin", [64, 128], F32)
        S.dma("sp", posin[:], I["nsa_cmp_pos"][l], writes=[posin])
        posT = S.sbp("posT", [128, 64], BF16)
        ps = C.psX.next()
        S.tr(ps[:, 0:64], posin[:], C.ident[0:64, 0:64], reads=[posin, C.ident], writes=[ps])
        S.copy("dve", posT[:], ps[:, 0:64], reads=[ps], writes=[posT])
        ck = S.sbp("ck", [128, 2], F32)
        ps = C.psX.next()
        for kv in range(2):
            for li in range(32):
                S.mm(ps[:, kv:kv + 1], cw[:, kv, li, :], posT[:, kv * 32 + li:kv * 32 + li + 1],
                     start=(li == 0), stop=(li == 31), reads=[cw, posT], writes=[ps])
        S.copy("dve", ck[:], ps[:, 0:2], reads=[ps], writes=[ck])
        kcmpT = S.sbp("kcmpT", [128, 2, 128], BF16)
        vcaug = S.sbp("vcaug", [128, 2, 161], BF16)
        vtmp = S.sbp("vtmp", [128, 128], BF16)
        S.op("dve", lambda: nc.vector.memset(kcmpT[:], 0.0), writes=[kcmpT])
        S.op("dve", lambda: nc.vector.memset(vtmp[:], 0.0), writes=[vtmp])
        for g in range(2):
            S.dma("pool", vcaug[:, g, 128:161], T["inter"], writes=[vcaug])
            for kv in range(2):
                src = kcin if kv == 0 else vcin
                ps = C.psX.next()
                for li in range(32):
                    S.mm(ps[:, 0:127], cw[:, kv, li, :], src[:, g, li:li + 2017:16],
                         start=(li == 0), stop=(li == 31), reads=[cw, src], writes=[ps])
                if kv == 0:
                    S.op("dve", lambda: nc.vector.tensor_scalar(out=kcmpT[:, g, 0:127], in0=ps[:, 0:127],
                                                                scalar1=ck[:, 0:1], scalar2=None, op0=ALU.add),
                         reads=[ps, ck], writes=[kcmpT])
                else:
                    S.op("dve", lambda: nc.vector.tensor_scalar(out=vtmp[:, 0:127], in0=ps[:, 0:127],
                                                                scalar1=ck[:, 1:2], scalar2=None, op0=ALU.add),
                         reads=[ps, ck], writes=[vtmp])
                    ps2 = C.psX.next()
                    psb = ps2[:].bitcast(BF16)
                    S.tr(psb[:, 0:128], vtmp[:], C.identb[:], reads=[vtmp, C.identb], writes=[ps2])
                    S.copy("dve", vcaug[:, g, 0:128], psb[:, 0:128], reads=[ps2], writes=[vcaug])

        qbuf = Ring([S.sbp("qA", [128, 8, 128], BF16) for _ in range(2)])
        ebuf = Ring([S.sbp("E", [128, 512], BF16) for _ in range(4)])
        pbuf = Ring([S.sbp("P", [128, 512], BF16) for _ in range(4)])
        ptb = Ring([S.sbp("PT", [128, 128], BF16) for _ in range(3)])
        small = Ring([S.sbp("sm", [128, 4], F32) for _ in range(8)])
        C.pvbuf = Ring([S.sbp("pvb", [128, 161], F32) for _ in range(8)])
        C.pvcols = 129
        oaccs = Ring([S.sbp("oacc", [128, 8, 128], F32) for _ in range(2)])
        imps = Ring([S.sbp("imp", [128, 2, 32], F32) for _ in range(2)])
        impf = Ring([S.sbp("impf", [128, 32], F32) for _ in range(2)])
        m8 = Ring([S.sbp("m8", [128, 8], F32) for _ in range(2)])
        selb = Ring([S.sbp("selb", [128, 128], F32) for _ in range(2)])
        for b_ in selb.bufs:
            S.op("dve", lambda: nc.vector.memset(b_[:], 0.0), writes=[b_])
        selbT = Ring([S.sbp("selbT", [128, 2, 4, 128], BF16) for _ in range(2)])
        for b_ in selbT.bufs:
            S.op("pool", lambda: nc.gpsimd.memset(b_[:], 0.0), writes=[b_])
        obs = Ring([S.sbp("ob", [128, 1024], BF16) for _ in range(2)])
        osts = Ring([S.sbp("ost", [128, 1024], BF16) for _ in range(2)])

        st_ = {}

        def prep(i):
            qt = qbuf.next()
            S.dma("sp", qt[:], R["qTA"][:, :, i * 128:(i + 1) * 128].rearrange("h p t -> p h t"), writes=[qt])
            oacc = oaccs.next()
            imp = imps.next()
            sT = selbT.next()
            off = 120 - 8 * i
            st_[i] = (qt, oacc, imp, sT, off)

        def cmp_g(i, g):
            qt, oacc, imp, sT, off = st_[i]
            ps = C.psS.next()
            for hh in range(4):
                S.mm(ps[:, hh * 128:(hh + 1) * 128], qt[:, 4 * g + hh, :], kcmpT[:, g, :],
                     reads=[qt, kcmpT], writes=[ps])
            E = ebuf.next()
            S.op("act", lambda: nc.scalar.activation(E[:], ps[:], AF.Exp, scale=SC_A), reads=[ps], writes=[E])
            P = pbuf.next()
            S.op("pool", lambda: nc.gpsimd.tensor_tensor(
                out=P[:].rearrange("p (a b) -> p a b", a=4), in0=E[:].rearrange("p (a b) -> p a b", a=4),
                in1=Gc[:, 4 * g:4 * g + 4, off:off + 128], op=ALU.mult), reads=[E, Gc], writes=[P])
            for hh in range(4):
                pst = C.psX.next()
                pstb = pst[:].bitcast(BF16)
                S.tr(pstb[:, 0:128], P[:, hh * 128:(hh + 1) * 128], C.identb[:], reads=[P, C.identb], writes=[pst])
                PT = ptb.next()
                S.copy(next(ev), PT[:], pstb[:, 0:128], reads=[pst], writes=[PT])
                S.mm(C.psV[hh][:, 0:161], PT[:], vcaug[:, g, :], reads=[PT, vcaug], writes=[C.psV[hh]])
            C.pvcols = 161
            dens, pvs = _post_bundle(C, 128, [(oacc[:, 4 * g + hh, :], oacc) for hh in range(4)],
                                     [(gt[:, i, 3 * (4 * g + hh):3 * (4 * g + hh) + 1], gt) for hh in range(4)],
                                     True, small)
            C.pvcols = 129
            for hh in range(4):
                h = 4 * g + hh
                den, pv = dens[hh], pvs[hh]
                if hh == 0:
                    S.op("dve", lambda: nc.vector.tensor_scalar(out=imp[:, g, :], in0=pv[:, 129:161],
                                                                scalar1=den[:, 1:2], scalar2=None, op0=ALU.mult),
                         reads=[pv, den], writes=[imp])
                else:
                    S.op("dve", lambda: nc.vector.scalar_tensor_tensor(
                        out=imp[:, g, :], in0=pv[:, 129:161], scalar=den[:, 1:2], in1=imp[:, g, :],
                        op0=ALU.mult, op1=ALU.add), reads=[pv, den, imp], writes=[imp])

        def sel_g(i, g):
            qt, oacc, imp, sT, off = st_[i]
            f = impf.next()
            S.op("dve", lambda: nc.vector.tensor_tensor(out=f[:], in0=imp[:, g, :], in1=selmul[:, i, :], op=ALU.mult),
                 reads=[imp, selmul], writes=[f])
            S.op("dve", lambda: nc.vector.tensor_tensor(out=f[:], in0=f[:], in1=seladd[:, i, :], op=ALU.add),
                 reads=[f, seladd], writes=[f])
            m = m8.next()
            S.op("dve", lambda: nc.vector.max(out=m[:], in_=f[:]), reads=[f], writes=[m])
            sb_ = selb.next()
            S.op("dve", lambda: nc.vector.tensor_scalar(out=sb_[:, 0:32], in0=f[:], scalar1=m[:, 7:8], scalar2=None,
                                                        op0=ALU.is_ge), reads=[f, m], writes=[sb_])
            S.op("dve", lambda: nc.vector.tensor_scalar(out=sb_[:, 0:32], in0=sb_[:, 0:32], scalar1=-1.0, scalar2=NEGB,
                                                        op0=ALU.add, op1=ALU.mult), reads=[sb_], writes=[sb_])
            pst = C.psX.next()
            S.tr(pst[:, 0:128], sb_[:], C.ident[:], reads=[sb_, C.ident], writes=[pst])
            for hh in range(4):
                S.copy(next(ev), sT[0:32, g, hh, :], pst[0:32, 0:128], reads=[pst], writes=[sT])

        def bundle(i, br, g):
            qt, oacc, imp, sT, off = st_[i]
            if br == 0:
                kts = list(range(0, i + 1))
                sel_emit = (lambda ps, kt, g=g: S.mm(
                    ps[:], esel[:, kt, :], sT[:, g].rearrange("p a b -> p (a b)"), start=True, stop=False,
                    reads=[esel, sT], writes=[ps]))
                mask_of = (lambda kt, g=g: (MA[:, g, i - kt], MA))
            else:
                kts = list(range(max(0, i - 4), i + 1))
                sel_emit = None
                mask_of = (lambda kt, g=g: (MA[:, g, (i - kt) if (i - kt) < 4 else 16], MA))
            qk_emit = (lambda ps, kt, first, g=g, br=br: S.mm(
                ps[:].rearrange("p (a b) -> p a b", a=4), kT[:, br, g, kt * 128:(kt + 1) * 128],
                qt[:, 4 * g:4 * g + 4, :], start=first, stop=True, reads=[kT, qt], writes=[ps]))
            v_of = (lambda kt, hh, g=g, br=br: (vaug[:, br, g, kt, :], vaug))
            _st_attention(C, kts, sel_emit, qk_emit, mask_of, v_of, 128, SC_A, ebuf, pbuf, par)
            _post_bundle(C, 128, [(oacc[:, 4 * g + hh, :], oacc) for hh in range(4)],
                         [(gt[:, i, 3 * (4 * g + hh) + 1 + br:3 * (4 * g + hh) + 2 + br], gt) for hh in range(4)],
                         False, small, defer=True)

        def finish(i):
            qt, oacc, imp, sT, off = st_.pop(i)
            _finish_tile(C, oacc, 8, obs.next(), osts.next(), R["oTA"], i, ev)

        prep(0)
        for g in range(2):
            cmp_g(0, g)
        for g in range(2):
            sel_g(0, g)
        for i in range(NT):
            nx = i + 1 < NT
            if nx:
                prep(i + 1)
            bundle(i, 0, 0)
            if nx:
                cmp_g(i + 1, 0)
            bundle(i, 0, 1)
            if nx:
                cmp_g(i + 1, 1)
            bundle(i, 1, 0)
            if nx:
                sel_g(i + 1, 0)
                sel_g(i + 1, 1)
            bundle(i, 1, 1)
            finish(i)


def _tables():
    T = {}
    T["t_ident"] = np.eye(128, dtype=np.float32)
    sp = np.arange(128)[:, None]
    tp = np.arange(128)[None, :]
    slA = np.array([2.0 ** (-(h + 1)) for h in range(8)])
    slC = np.array([2.0 ** (-2.0 * (h + 1)) for h in range(4)])

    def toep(slope, delta, wcut=None):
        u = (delta + tp - sp).astype(np.float64)
        v = np.exp(-slope * np.maximum(u, 0.0))
        v = np.where(u >= 0, v, 0.0)
        if wcut is not None:
            v = np.where(u < wcut, v, 0.0)
        return v

    mA = np.zeros((128, 2, 17, 4, 128), np.float64)
    for g in range(2):
        for hh in range(4):
            s = slA[4 * g + hh]
            for di in range(16):
                mA[:, g, di, hh, :] = toep(s, 128 * di)
            mA[:, g, 16, hh, :] = toep(s, 512, 512)
    T["t_maskA"] = mA.reshape(128, -1).astype(np.float32)
    mB = np.zeros((128, 2, 2, 4, 128), np.float64)
    for g in range(2):
        for hh in range(4):
            s = slA[4 * g + hh]
            mB[:, g, 0, hh, :] = toep(s, 0, 128)
            mB[:, g, 1, hh, :] = toep(s, 128, 128)
    T["t_maskB"] = mB.reshape(128, -1).astype(np.float32)
    mC = np.zeros((128, 16, 4, 128), np.float64)
    for di in range(16):
        for h in range(4):
            mC[:, di, h, :] = toep(slC[h], 128 * di)
    T["t_maskC"] = mC.reshape(128, -1).astype(np.float32)
    tq = np.arange(128)[:, None]
    m = (np.arange(248) - 120)[None, :]
    dist = (tq - 16 * m - 31).astype(np.float64)
    G = np.zeros((128, 8, 248), np.float64)
    for h in range(8):
        G[:, h, :] = np.where(dist >= 0, np.exp(-slA[h] * np.maximum(dist, 0.0)), 0.0)
    T["t_gcmp"] = G.reshape(128, -1).astype(np.float32)
    n_cmp = 127
    c_start = 16 * np.arange(n_cmp)
    s_start = 64 * np.arange(32)
    inter = np.clip(np.minimum(c_start[:, None] + 32, s_start[None, :] + 64)
                    - np.maximum(c_start[:, None], s_start[None, :]), 0, None) / 32.0
    ia = np.zeros((128, 33), np.float32)
    ia[:127, 0] = 1.0
    ia[:127, 1:] = inter
    T["t_inter"] = ia
    t = (128 * np.arange(NT)[None, :, None] + np.arange(128)[:, None, None])
    j = np.arange(32)[None, None, :]
    blk = t // 64
    forced = (j == 0) | (j == blk) | (j == blk - 1)
    valid = j <= blk
    T["t_selmul"] = (valid & ~forced).astype(np.float32).reshape(128, -1)
    T["t_seladd"] = np.where(forced, BIG, np.where(valid, 0.0, -BIG)).astype(np.float32).reshape(128, -1)
    n8 = np.arange(8)[None, None, None, :]
    t4 = t[:, :, :, None] + np.zeros((1, 1, 4, 1), np.int64)
    blk_c = t4 // 256
    past = n8 < blk_c
    T["t_cmul"] = past.astype(np.float32).reshape(128, -1)
    T["t_cadd"] = np.where(past, 0.0, -BIG).astype(np.float32).reshape(128, -1)
    T["t_cown"] = (n8 == blk_c).astype(np.float32).reshape(128, -1)
    eA = np.zeros((32, NT, 128), np.float32)
    for kt in range(NT):
        for s in range(128):
            eA[2 * kt + s // 64, kt, s] = 1.0
    T["t_eselA"] = eA.reshape(32, -1)
    eC = np.zeros((8, NT, 128), np.float32)
    for kt in range(NT):
        eC[kt // 2, kt, :] = 1.0
    T["t_eselC"] = eC.reshape(8, -1)
    oh = np.zeros((32, 32, 128), np.float32)
    for e in range(32):
        oh[e, e, :] = 1.0
    T["t_onehot"] = oh.reshape(32, -1)
    return T


def _prep_inputs(inputs):
    f = lambda a: np.ascontiguousarray(np.asarray(a, dtype=np.float32))
    common = {}
    for nm in ("w_in", "nsa_cmp_w", "sink_b", "w_br_a", "w_br_b", "w_br_c", "w_out",
               "ln1_g", "ln1_b", "ln2_g", "ln2_b", "w_gate", "w_up", "w_down"):
        common[nm] = f(inputs[nm])
    common["nsa_cmp_pos"] = f(inputs["nsa_cmp_pos"]).reshape(DEPTH, 64, 128)
    common["w_rt"] = np.ascontiguousarray(np.concatenate([f(inputs["w_group"]), f(inputs["w_router"])], axis=-1))
    common["b_rt"] = np.ascontiguousarray(np.concatenate([f(inputs["b_group"]), f(inputs["b_router"])], axis=-1))
    common.update(_tables())
    x = f(inputs["x"])
    in_maps = []
    for c in range(8):
        d = dict(common)
        d["x"] = np.ascontiguousarray(x[c % 4])
        in_maps.append(d)
    return in_maps


def kernel(**inputs):
    nc, used = build_program()
    in_maps = [{k: m[k] for k in used} for m in _prep_inputs(inputs)]
    res = run_bass_kernel_spmd(nc, in_maps, core_ids=list(range(8)))
    out = np.stack([np.asarray(res.results[c]["out"], dtype=np.float32) for c in range(4)], axis=0)
    return out


def phase_mixB(C):
    nc, S, I, R, T = C.nc, C.S, C.I, C.R, C.T
    l = C.l
    ev = _evac_engines()
    par = [0]
    with S.phase():
        MB = S.sbp("MB", [128, 2, 2, 4, 128], BF16)
        S.dma("pool", MB[:].rearrange("p g a b c -> p (g a b) c"),
              T["maskB"].rearrange("p (a c) -> p a c", c=128), writes=[MB])
        kT = S.sbp("kTB", [128, 2, TOK], BF16)
        S.op("pool", lambda: nc.gpsimd.memset(kT[:], 0.0), writes=[kT])
        for g in range(2):
            S.dma("sp", kT[0:64, g, :], R["kTB"][g], writes=[kT])
        vaug = S.sbp("vaugB", [128, 2, NT, 65], BF16)
        S.op("pool", lambda: nc.gpsimd.memset(vaug[:], 1.0), writes=[vaug])
        for g in range(2):
            S.dma("sp", vaug[:, g, :, 0:64],
                  R["vB"][:, g * 64:(g + 1) * 64].rearrange("(kt p) d -> p kt d", p=128), writes=[vaug])
        snk = S.sbp("snk", [128, 8], F32)
        S.dma("sp", snk[:], I["sink_b"][l].partition_broadcast(128), writes=[snk])
        esnk = S.sbp("esnk", [128, 8], F32)
        S.op("act", lambda: nc.scalar.activation(esnk[:], snk[:], AF.Exp), reads=[snk], writes=[esnk])
        qbuf = Ring([S.sbp("qB", [128, 8, 128], BF16) for _ in range(2)])
        for b_ in qbuf.bufs:
            S.op("pool", lambda: nc.gpsimd.memset(b_[:], 0.0), writes=[b_])
        ebuf = Ring([S.sbp("E", [128, 512], BF16) for _ in range(4)])
        pbuf = Ring([S.sbp("P", [128, 512], BF16) for _ in range(4)])
        small = Ring([S.sbp("sm", [128, 4], F32) for _ in range(8)])
        C.pvbuf = Ring([S.sbp("pvb", [128, 161], F32) for _ in range(8)])
        C.pvcols = 65
        oaccs = Ring([S.sbp("oaccB", [128, 8, 64], F32) for _ in range(2)])
        obs = Ring([S.sbp("ob", [128, 512], BF16) for _ in range(2)])
        osts = Ring([S.sbp("ost", [128, 512], BF16) for _ in range(2)])
        for i in range(NT):
            qt = qbuf.next()
            S.dma("sp", qt[0:64], R["qTB"][:, :, i * 128:(i + 1) * 128].rearrange("h p t -> p h t"), writes=[qt])
            oacc = oaccs.next()
            for g in range(2):
                kts = list(range(max(0, i - 1), i + 1))
                qk_emit = (lambda ps, kt, first, g=g: S.mm(
                    ps[:].rearrange("p (a b) -> p a b", a=4), kT[:, g, kt * 128:(kt + 1) * 128],
                    qt[:, 4 * g:4 * g + 4, :], start=first, stop=True, reads=[kT, qt], writes=[ps]))
                mask_of = (lambda kt, g=g: (MB[:, g, i - kt], MB))
                v_of = (lambda kt, hh, g=g: (vaug[:, g, kt, :], vaug))
                _st_attention(C, kts, None, qk_emit, mask_of, v_of, 64, SC_B, ebuf, pbuf, par)
                _post_bundle(C, 64, [(oacc[:, 4 * g + hh, :], oacc) for hh in range(4)], None, True, small,
                             extras=[(esnk[:, 4 * g + hh:4 * g + hh + 1], esnk) for hh in range(4)], defer=True)
            _finish_tile(C, oacc, 4, obs.next(), osts.next(), R["oTB"], i, ev)


def phase_mixC(C):
    nc, S, I, R, T = C.nc, C.S, C.I, C.R, C.T
    ev = _evac_engines()
    par = [0]
    with S.phase():
        MC = S.sbp("MC", [128, 16, 4, 128], BF16)
        S.dma("pool", MC[:].rearrange("p a b c -> p (a b) c"),
              T["maskC"].rearrange("p (a c) -> p a c", c=128), writes=[MC])
        kT = S.sbp("kTC", [128, 4, TOK], BF16)
        for h in range(4):
            S.dma("sp", kT[:, h, :], R["kTC"][h], writes=[kT])
        vaug = S.sbp("vaugC", [128, 4, NT, 129], BF16)
        S.op("pool", lambda: nc.gpsimd.memset(vaug[:], 1.0), writes=[vaug])
        for h in range(4):
            S.dma("sp", vaug[:, h, :, 0:128],
                  R["vC"][:, h * 128:(h + 1) * 128].rearrange("(kt p) d -> p kt d", p=128), writes=[vaug])
        cmul = S.sbp("cmul", [128, NT, 32], F32)
        cadd = S.sbp("cadd", [128, NT, 32], F32)
        cown = S.sbp("cown", [128, NT, 32], F32)
        for t_, nm in ((cmul, "cmul"), (cadd, "cadd"), (cown, "cown")):
            S.dma("sp", t_[:], T[nm].rearrange("p (i c) -> p i c", c=32), writes=[t_])
        esel = S.sbp("eselC", [128, NT, 128], BF16)
        S.op("pool", lambda: nc.gpsimd.memset(esel[:], 0.0), writes=[esel])
        S.dma("pool", esel[0:8], T["eselC"].rearrange("p (i c) -> p i c", c=128), writes=[esel])
        kmf = S.sbp("kmf", [128, 4, 8], F32)
        for h in range(4):
            S.op("dve", lambda: nc.vector.tensor_reduce(
                out=kmf[:, h, :], in_=kT[:, h, :].rearrange("p (n s) -> p n s", s=256), axis=AX.X, op=ALU.add),
                reads=[kT], writes=[kmf])
        kmT = S.sbp("kmT", [128, 4, 8], BF16)
        S.op("dve", lambda: nc.vector.tensor_scalar(out=kmT[:], in0=kmf[:], scalar1=1.0 / 256.0, scalar2=None,
                                                    op0=ALU.mult), reads=[kmf], writes=[kmT])
        qbuf = Ring([S.sbp("qC", [128, 4, 128], BF16) for _ in range(2)])
        ebuf = Ring([S.sbp("E", [128, 512], BF16) for _ in range(4)])
        pbuf = Ring([S.sbp("P", [128, 512], BF16) for _ in range(4)])
        small = Ring([S.sbp("sm", [128, 4], F32) for _ in range(8)])
        C.pvbuf = Ring([S.sbp("pvb", [128, 161], F32) for _ in range(8)])
        C.pvcols = 129
        oaccs = Ring([S.sbp("oaccC", [128, 4, 128], F32) for _ in range(2)])
        scf = Ring([S.sbp("scf", [128, 32], F32) for _ in range(2)])
        m8 = Ring([S.sbp("m8c", [128, 4, 8], F32) for _ in range(2)])
        selb = Ring([S.sbp("selbc", [128, 32], F32) for _ in range(2)])
        selbT = Ring([S.sbp("selbTc", [128, 4, 128], BF16) for _ in range(2)])
        for b_ in selbT.bufs:
            S.op("pool", lambda: nc.gpsimd.memset(b_[:], 0.0), writes=[b_])
        obs = Ring([S.sbp("ob", [128, 512], BF16) for _ in range(2)])
        osts = Ring([S.sbp("ost", [128, 512], BF16) for _ in range(2)])
        for i in range(NT):
            qt = qbuf.next()
            S.dma("sp", qt[:], R["qTC"][:, :, i * 128:(i + 1) * 128].rearrange("h p t -> p h t"), writes=[qt])
            oacc = oaccs.next()
            ps = C.psX.next()
            for h in range(4):
                S.mm(ps[:, h * 8:(h + 1) * 8], qt[:, h, :], kmT[:, h, :], reads=[qt, kmT], writes=[ps])
            f = scf.next()
            S.op("dve", lambda: nc.vector.tensor_tensor(out=f[:], in0=ps[:, 0:32], in1=cmul[:, i, :], op=ALU.mult),
                 reads=[ps, cmul], writes=[f])
            S.op("dve", lambda: nc.vector.tensor_tensor(out=f[:], in0=f[:], in1=cadd[:, i, :], op=ALU.add),
                 reads=[f, cadd], writes=[f])
            m = m8.next()
            sb_ = selb.next()
            for h in range(4):
                S.op("dve", lambda: nc.vector.max(out=m[:, h, :], in_=f[:, h * 8:(h + 1) * 8]), reads=[f], writes=[m])
                S.op("dve", lambda: nc.vector.tensor_scalar(out=sb_[:, h * 8:(h + 1) * 8], in0=f[:, h * 8:(h + 1) * 8],
                                                            scalar1=m[:, h, 2:3], scalar2=None, op0=ALU.is_ge),
                     reads=[f, m], writes=[sb_])
            S.op("dve", lambda: nc.vector.tensor_tensor(out=sb_[:], in0=sb_[:], in1=cown[:, i, :], op=ALU.max),
                 reads=[sb_, cown], writes=[sb_])
            S.op("dve", lambda: nc.vector.tensor_scalar(out=sb_[:], in0=sb_[:], scalar1=-1.0, scalar2=NEGB,
                                                        op0=ALU.add, op1=ALU.mult), reads=[sb_], writes=[sb_])
            pst = C.psX.next()
            for h in range(4):
                S.tr(pst[0:8, h * 128:(h + 1) * 128], sb_[:, h * 8:(h + 1) * 8], C.ident[:],
                     reads=[sb_, C.ident], writes=[pst])
            sT = selbT.next()
            S.copy(next(ev), sT[0:8].rearrange("p a b -> p (a b)"), pst[0:8, :], reads=[pst], writes=[sT])
            kts = list(range(0, i + 1))
            sel_emit = (lambda ps, kt: S.mm(ps[:], esel[:, kt, :], sT[:].rearrange("p a b -> p (a b)"),
                                            start=True, stop=False, reads=[esel, sT], writes=[ps]))

            def qk_emit(ps, kt, first):
                for h in range(4):
                    S.mm(ps[:, h * 128:(h + 1) * 128], kT[:, h, kt * 128:(kt + 1) * 128], qt[:, h, :],
                         start=False, stop=(h == 3), reads=[kT, qt], writes=[ps])
            mask_of = (lambda kt: (MC[:, i - kt], MC))
            v_of = (lambda kt, hh: (vaug[:, hh, kt, :], vaug))
            _st_attention(C, kts, sel_emit, qk_emit, mask_of, v_of, 128, SC_A, ebuf, pbuf, par)
            _post_bundle(C, 128, [(oacc[:, hh, :], oacc) for hh in range(4)], None, True, small, defer=True)
            _finish_tile(C, oacc, 4, obs.next(), osts.next(), R["oTC"], i, ev)


def _layernorm_tile(C, y, g_sb, b_sb, xn, out_t, st, mv, part=0):
    nc, S = C.nc, C.S
    for c in range(4):
        S.op("dve", lambda: nc.vector.bn_stats(st[:, c, :], y[:, c * 512:(c + 1) * 512]), reads=[y], writes=[st])
    S.op("dve", lambda: nc.vector.bn_aggr(mv[:, 0:2], st[:].rearrange("p a b -> p (a b)")), reads=[st], writes=[mv])
    S.op("dve", lambda: nc.vector.tensor_scalar(out=mv[:, 2:3], in0=mv[:, 1:2], scalar1=LN_EPS, scalar2=None,
                                                op0=ALU.add), reads=[mv], writes=[mv])
    S.op("act", lambda: nc.scalar.activation(mv[:, 3:4], mv[:, 2:3], AF.Sqrt), reads=[mv], writes=[mv])
    if part == 1:
        return
    _ln_part2(C, y, g_sb, b_sb, xn, out_t, mv)


def _ln_part2(C, y, g_sb, b_sb, xn, out_t, mv):
    nc, S = C.nc, C.S
    S.op("dve", lambda: nc.vector.reciprocal(mv[:, 4:5], mv[:, 3:4]), reads=[mv], writes=[mv])
    S.op("dve", lambda: nc.vector.tensor_scalar(out=mv[:, 5:6], in0=mv[:, 0:1], scalar1=-1.0, scalar2=mv[:, 4:5],
                                                op0=ALU.mult, op1=ALU.mult), reads=[mv], writes=[mv])
    S.op("act", lambda: nc.scalar.activation(xn[:], y[:], AF.Identity, bias=mv[:, 5:6], scale=mv[:, 4:5]),
         reads=[y, mv], writes=[xn])
    S.op("dve", lambda: nc.vector.tensor_tensor(out=xn[:], in0=xn[:], in1=g_sb[:], op=ALU.mult),
         reads=[xn, g_sb], writes=[xn])
    S.op("pool", lambda: nc.gpsimd.tensor_tensor(out=out_t[:], in0=xn[:], in1=b_sb[:], op=ALU.add),
         reads=[xn, b_sb], writes=[out_t])


def phase_merge(C, x_in, blk):
    nc, S, I, R, T = C.nc, C.S, C.I, C.R, C.T
    l = C.l
    ev = _evac_engines()
    t0 = blk * 1024
    with S.phase():
        mT = S.sbp("mT", [128, KC, 1024], BF16)
        wo = S.sbp("wo", [128, KC, D], BF16)

        def load_wo():
            for db in range(4):
                S.dma("pool", wo[:, :, db * 512:(db + 1) * 512],
                      I["w_out"][l, :, db * 512:(db + 1) * 512].rearrange("(kc p) c -> p kc c", p=128), writes=[wo])

        with S.phase():
            xT = S.sbp("xTm", [128, KC, 1024], BF16)
            S.dma("sp", xT[:], R["xT"][:, :, t0:t0 + 1024].rearrange("k p t -> p k t"), writes=[xT])
            oT = S.sbp("oT", [128, 16, 1024], BF16)
            S.dma("sp", oT[:, 0:8, :], R["oTA"][:, :, t0:t0 + 1024].rearrange("k p t -> p k t"), writes=[oT])
            S.dma("sp", oT[:, 8:12, :], R["oTB"][:, :, t0:t0 + 1024].rearrange("k p t -> p k t"), writes=[oT])
            S.dma("sp", oT[:, 12:16, :], R["oTC"][:, :, t0:t0 + 1024].rearrange("k p t -> p k t"), writes=[oT])
            wgs = Ring([S.sbp("wg", [128, KC, 3, 128], BF16) for _ in range(2)])
            wbs = Ring([S.sbp("wb", [128, 16, 128], BF16) for _ in range(2)])
            sgs = Ring([S.sbp("sgb", [128, 512], BF16) for _ in range(3)])
            tmps = Ring([S.sbp("tmpm", [128, 512], F32) for _ in range(3)])
            maccs = Ring([S.sbp("macc", [128, 512], F32) for _ in range(2)])
            kranges = ((0, 8), (8, 12), (12, 16))
            def load_mw(dc):
                wgt = wgs.next()
                for gi in range(3):
                    c0 = OFF_MG + gi * 2048 + dc * 128
                    S.dma("pool", wgt[:, :, gi, :], I["w_in"][l, :, c0:c0 + 128].rearrange("(kc p) c -> p kc c", p=128),
                          writes=[wgt])
                wbt = wbs.next()
                S.dma("pool", wbt[:, 0:8, :], I["w_br_a"][l, :, dc * 128:(dc + 1) * 128].rearrange("(k p) c -> p k c", p=128),
                      writes=[wbt])
                S.dma("pool", wbt[:, 8:12, :], I["w_br_b"][l, :, dc * 128:(dc + 1) * 128].rearrange("(k p) c -> p k c", p=128),
                      writes=[wbt])
                S.dma("pool", wbt[:, 12:16, :], I["w_br_c"][l, :, dc * 128:(dc + 1) * 128].rearrange("(k p) c -> p k c", p=128),
                      writes=[wbt])
                return wgt, wbt

            nxt = load_mw(0)
            for dc in range(16):
                wgt, wbt = nxt
                if dc + 1 < 16:
                    nxt = load_mw(dc + 1)
                if dc == 1:
                    load_wo()
                for tb in range(2):
                    macc = maccs.next()
                    for gi in range(3):
                        psg = C.psS.next()
                        for kc in range(KC):
                            S.mm(psg[:], wgt[:, kc, gi, :], xT[:, kc, tb * 512:(tb + 1) * 512],
                                 start=(kc == 0), stop=(kc == KC - 1), reads=[wgt, xT], writes=[psg])
                        sgb = sgs.next()
                        S.op("act", lambda: nc.scalar.activation(sgb[:], psg[:], AF.Sigmoid), reads=[psg], writes=[sgb])
                        psb = C.psX.next()
                        k0, k1 = kranges[gi]
                        for k in range(k0, k1):
                            S.mm(psb[:], wbt[:, k, :], oT[:, k, tb * 512:(tb + 1) * 512],
                                 start=(k == k0), stop=(k == k1 - 1), reads=[wbt, oT], writes=[psb])
                        if gi == 0:
                            S.op("dve", lambda: nc.vector.tensor_tensor(out=macc[:], in0=psb[:], in1=sgb[:], op=ALU.mult),
                                 reads=[psb, sgb], writes=[macc])
                        else:
                            tmp = tmps.next()
                            S.op("dve", lambda: nc.vector.tensor_tensor(out=tmp[:], in0=psb[:], in1=sgb[:], op=ALU.mult),
                                 reads=[psb, sgb], writes=[tmp])
                            dst = macc[:] if gi == 1 else mT[:, dc, tb * 512:(tb + 1) * 512]
                            S.op("pool", lambda: nc.gpsimd.tensor_tensor(out=dst, in0=macc[:], in1=tmp[:], op=ALU.add),
                                 reads=[macc, tmp], writes=[macc] if gi == 1 else [mT])
        if "mg1" in C.dbg:
            return
        with S.phase():
            lng = S.sbp("lng", [128, D], F32)
            lnb = S.sbp("lnb", [128, D], F32)
            S.dma("sp", lng[:], I["ln1_g"][l].partition_broadcast(128), writes=[lng])
            S.dma("sp", lnb[:], I["ln1_b"][l].partition_broadcast(128), writes=[lnb])
            wr = S.sbp("wr", [128, KC, 36], F32)
            S.dma("sp", wr[:], I["w_rt"][l].rearrange("(p kc) c -> p kc c", kc=KC), writes=[wr])
            brt = S.sbp("brt", [128, 36], F32)
            S.dma("sp", brt[:], I["b_rt"][l].partition_broadcast(128), writes=[brt])
            xts = Ring([S.sbp("xt", [128, D], F32) for _ in range(2)])
            ys = Ring([S.sbp("y", [128, D], F32) for _ in range(2)])
            xns = Ring([S.sbp("xn", [128, D], F32) for _ in range(1)])
            x1s = Ring([S.sbp("x1t", [128, D], F32) for _ in range(2)])
            sts = Ring([S.sbp("st", [128, 4, 6], F32) for _ in range(2)])
            mvs = Ring([S.sbp("mv", [128, 8], F32) for _ in range(2)])
            x1Tb = Ring([S.sbp("x1Tb", [128, KC, 128], BF16) for _ in range(1)])
            x1Tf = Ring([S.sbp("x1Tf", [128, KC, 128], F32) for _ in range(2)])
            rts = Ring([S.sbp("rt", [128, 128], F32) for _ in range(3)])
            cTs = Ring([S.sbp("cTst", [32, 128], BF16) for _ in range(2)])
            x1_of = {}

            def stage_a(ti):
                i = blk * 8 + ti
                xt = xts.next()
                S.dma("sp", xt[:], x_in[i * 128:(i + 1) * 128, :], writes=[xt])
                y = ys.next()
                for db in range(4):
                    ps = C.psV[db]
                    for kc in range(KC):
                        S.mm(ps[:], mT[:, kc, ti * 128:(ti + 1) * 128], wo[:, kc, db * 512:(db + 1) * 512],
                             start=(kc == 0), stop=(kc == KC - 1), reads=[mT, wo], writes=[ps])
                    S.op("dve", lambda: nc.vector.scalar_tensor_tensor(
                        out=y[:, db * 512:(db + 1) * 512], in0=xt[:, db * 512:(db + 1) * 512], scalar=ALPHA, in1=ps[:],
                        op0=ALU.mult, op1=ALU.add), reads=[xt, ps], writes=[y])
                x1t = x1s.next()
                _layernorm_tile(C, y, lng, lnb, xns.next(), x1t, sts.next(), mvs.next())
                S.dma("sp", R["x1"][i * 128:(i + 1) * 128, :], x1t[:], reads=[x1t])
                x1_of[ti] = x1t

            xf_of, rt_of = {}, {}

            def stage_b1(ti):
                i = blk * 8 + ti
                x1t = x1_of.pop(ti)
                xb_ = x1Tb.next()
                xf_ = x1Tf.next()
                for q in range(4):
                    ps = C.psX.next()
                    for j in range(4):
                        kc = q * 4 + j
                        S.tr(ps[:, j * 128:(j + 1) * 128], x1t[:, kc:D:KC], C.ident[:],
                             reads=[x1t, C.ident], writes=[ps])
                    S.copy("act", xf_[:, q * 4:(q + 1) * 4, :], ps[:].rearrange("p (a b) -> p a b", a=4),
                           reads=[ps], writes=[xf_])
                    S.copy("dve", xb_[:, q * 4:(q + 1) * 4, :], xf_[:, q * 4:(q + 1) * 4, :],
                           reads=[xf_], writes=[xb_])
                if "mg5" not in C.dbg:
                    S.dma("sp", R["x1T"][:, :, i * 128:(i + 1) * 128].rearrange("k p t -> p k t"), xb_[:], reads=[xb_])
                xf_of[ti] = xf_

            def stage_b2(ti):
                i = blk * 8 + ti
                xf_ = xf_of.pop(ti)
                psr = C.psS.next()
                for kc in range(KC):
                    S.mm(psr[:, 0:36], xf_[:, kc, :], wr[:, kc, :], start=(kc == 0), stop=(kc == KC - 1),
                         reads=[xf_, wr], writes=[psr])
                rt = rts.next()
                LG, GM, GN, EM, T8, SL, SC = 0, 36, 40, 44, 76, 84, 116
                dv = lambda f, rd, wr_: S.op("dve", f, reads=rd, writes=wr_)
                dv(lambda: nc.vector.tensor_tensor(out=rt[:, LG:LG + 36], in0=psr[:, 0:36], in1=brt[:], op=ALU.add),
                   [psr, brt], [rt])
                dv(lambda: nc.vector.tensor_reduce(out=rt[:, SC:SC + 1], in_=rt[:, LG:LG + 4], axis=AX.X, op=ALU.max),
                   [rt], [rt])
                dv(lambda: nc.vector.tensor_scalar(out=rt[:, SC + 1:SC + 2], in0=rt[:, SC:SC + 1], scalar1=-1.0,
                                                   scalar2=None, op0=ALU.mult), [rt], [rt])
                S.op("act", lambda: nc.scalar.activation(rt[:, SC + 8:SC + 12], rt[:, LG:LG + 4], AF.Exp,
                                                         bias=rt[:, SC + 1:SC + 2], scale=1.0), reads=[rt], writes=[rt])
                dv(lambda: nc.vector.tensor_reduce(out=rt[:, SC + 2:SC + 3], in_=rt[:, SC + 8:SC + 12], axis=AX.X,
                                                   op=ALU.add), [rt], [rt])
                dv(lambda: nc.vector.reciprocal(rt[:, SC + 3:SC + 4], rt[:, SC + 2:SC + 3]), [rt], [rt])
                dv(lambda: nc.vector.tensor_scalar(out=rt[:, GM:GM + 4], in0=rt[:, LG:LG + 4], scalar1=rt[:, SC:SC + 1],
                                                   scalar2=None, op0=ALU.is_ge), [rt], [rt])
                dv(lambda: nc.vector.tensor_scalar(out=rt[:, GN:GN + 4], in0=rt[:, GM:GM + 4], scalar1=-1.0, scalar2=BIG,
                                                   op0=ALU.add, op1=ALU.mult), [rt], [rt])
                for g in range(4):
                    dv(lambda: nc.vector.tensor_scalar(
                        out=rt[:, EM + 8 * g:EM + 8 * g + 8], in0=rt[:, LG + 4 + 8 * g:LG + 12 + 8 * g],
                        scalar1=rt[:, GM + g:GM + g + 1], scalar2=rt[:, GN + g:GN + g + 1],
                        op0=ALU.mult, op1=ALU.add), [rt], [rt])
                dv(lambda: nc.vector.max(out=rt[:, T8:T8 + 8], in_=rt[:, EM:EM + 32]), [rt], [rt])
                dv(lambda: nc.vector.tensor_scalar(out=rt[:, SL:SL + 32], in0=rt[:, EM:EM + 32],
                                                   scalar1=rt[:, T8 + 1:T8 + 2], scalar2=None, op0=ALU.is_ge), [rt], [rt])
                dv(lambda: nc.vector.tensor_scalar(out=rt[:, SC + 4:SC + 5], in0=rt[:, T8:T8 + 1], scalar1=-1.0,
                                                   scalar2=None, op0=ALU.mult), [rt], [rt])
                dv(lambda: nc.vector.tensor_scalar(out=rt[:, EM:EM + 32], in0=rt[:, EM:EM + 32], scalar1=-1.0e4,
                                                   scalar2=None, op0=ALU.max), [rt], [rt])
                S.op("act", lambda: nc.scalar.activation(rt[:, EM:EM + 32], rt[:, EM:EM + 32], AF.Exp,
                                                         bias=rt[:, SC + 4:SC + 5], scale=1.0), reads=[rt], writes=[rt])
                S.op("act", lambda: nc.scalar.activation(rt[:, SC + 5:SC + 6], rt[:, T8 + 1:T8 + 2], AF.Exp,
                                                         bias=rt[:, SC + 4:SC + 5], scale=1.0), reads=[rt], writes=[rt])
                dv(lambda: nc.vector.tensor_scalar(out=rt[:, SC + 5:SC + 6], in0=rt[:, SC + 5:SC + 6], scalar1=1.0,
                                                   scalar2=None, op0=ALU.add), [rt], [rt])
                dv(lambda: nc.vector.reciprocal(rt[:, SC + 6:SC + 7], rt[:, SC + 5:SC + 6]), [rt], [rt])
                dv(lambda: nc.vector.tensor_tensor(out=rt[:, SC + 7:SC + 8], in0=rt[:, SC + 6:SC + 7],
                                                   in1=rt[:, SC + 3:SC + 4], op=ALU.mult), [rt], [rt])
                dv(lambda: nc.vector.tensor_tensor(out=rt[:, SL:SL + 32], in0=rt[:, SL:SL + 32], in1=rt[:, EM:EM + 32],
                                                   op=ALU.mult), [rt], [rt])
                dv(lambda: nc.vector.tensor_scalar(out=rt[:, SL:SL + 32], in0=rt[:, SL:SL + 32],
                                                   scalar1=rt[:, SC + 7:SC + 8], scalar2=None, op0=ALU.mult), [rt], [rt])
                rt_of[ti] = rt

            def stage_b3(ti):
                i = blk * 8 + ti
                rt = rt_of.pop(ti)
                LG, GM, GN, EM, T8, SL, SC = 0, 36, 40, 44, 76, 84, 116
                pst = C.psX.next()
                S.tr(pst[0:32, 0:128], rt[:, SL:SL + 32], C.ident[:], reads=[rt, C.ident], writes=[pst])
                cst = cTs.next()
                S.copy("act", cst[:], pst[0:32, 0:128], reads=[pst], writes=[cst])
                S.dma("sp", R["cT"][:, i * 128:(i + 1) * 128], cst[:], reads=[cst])

            for k in range(-2, 9):
                if 0 <= k + 2 < 8:
                    stage_a(k + 2)
                if 0 <= k + 1 < 8:
                    stage_b1(k + 1)
                if 0 <= k < 8:
                    stage_b2(k)
                if 0 <= k - 1 < 8:
                    stage_b3(k - 1)


def phase_moe(C, blk, x_out):
    nc, S, I, R, T = C.nc, C.S, C.I, C.R, C.T
    l = C.l
    ev = _evac_engines()
    t0 = blk * 1024
    with S.phase():
      yacc = S.sbp("yacc", [128, 8, D], F32)
      yparts = [[S.tok("yp") for _ in range(4)] for _ in range(8)]
      with S.phase():
        x1T = S.sbp("x1T", [128, KC, 1024], BF16)
        S.dma("sp", x1T[:], R["x1T"][:, :, t0:t0 + 1024].rearrange("k p t -> p k t"), writes=[x1T])
        cT = S.sbp("cT", [128, 1024], BF16)
        S.op("pool", lambda: nc.gpsimd.memset(cT[:], 0.0), writes=[cT])
        S.dma("sp", cT[0:32], R["cT"][:, t0:t0 + 1024], writes=[cT])
        oh = S.sbp("oh", [128, 32, 128], BF16)
        S.op("pool", lambda: nc.gpsimd.memset(oh[:], 0.0), writes=[oh])
        S.dma("pool", oh[0:32], T["onehot"].rearrange("p (e c) -> p e c", c=128), writes=[oh])
        wgus = Ring([S.sbp("wgu", [128, 2, KC, 256], BF16) for _ in range(2)])
        wds = Ring([S.sbp("wd", [128, 2, 2, D], BF16) for _ in range(2)])
        aTs = Ring([S.sbp("aT", [128, 2, 2, 1024], BF16) for _ in range(2)])
        cbs = Ring([S.sbp("cb", [128, 1024], BF16) for _ in range(2)])
        sgs = Ring([S.sbp("sg", [128, 512], BF16) for _ in range(3)])
        t1s = Ring([S.sbp("t1", [128, 512], BF16) for _ in range(3)])
        psH = Ring([C.psS.bufs[0], C.psS.bufs[1], C.psX.bufs[0], C.psX.bufs[1]])
        psC = psH
        psY = Ring([C.psV[0], C.psV[1], C.psV[2], C.psV[3]])
        def load_gu(e):
            w = wgus.next()
            S.dma("pool", w[:, 0].rearrange("p (a k) f -> p a (k f)", a=2),
                  I["w_gate"][l, e].rearrange("(p a k) f -> p a (k f)", a=2, k=KC // 2), writes=[w])
            S.dma("pool", w[:, 1].rearrange("p (a k) f -> p a (k f)", a=2),
                  I["w_up"][l, e].rearrange("(p a k) f -> p a (k f)", a=2, k=KC // 2), writes=[w])
            return w

        def load_d(ep):
            wdt = wds.next()
            for j in range(2):
                S.dma("pool", wdt[:, j], I["w_down"][l, 2 * ep + j].rearrange("(p fc) d -> p fc d", fc=2), writes=[wdt])
            return wdt

        nxt_w = load_gu(0)
        nxt_d = load_d(0)
        for ep in range(16):
            wdt = nxt_d
            a = aTs.next()
            for j in range(2):
                e = 2 * ep + j
                w = nxt_w
                if e + 1 < 32:
                    nxt_w = load_gu(e + 1)
                if j == 0 and ep + 1 < 16:
                    nxt_d = load_d(ep + 1)
                cbt = cbs.next()
                for tb in range(2):
                    psc = psC.next()
                    S.mm(psc[:], oh[:, e, :], cT[:, tb * 512:(tb + 1) * 512], reads=[oh, cT], writes=[psc])
                    S.copy("act", cbt[:, tb * 512:(tb + 1) * 512], psc[:], reads=[psc], writes=[cbt])
                for fc in range(2):
                    for tb in range(2):
                        psg = psH.next()
                        for kc in range(KC):
                            S.mm(psg[:], w[:, 0, kc, fc:256:2], x1T[:, kc, tb * 512:(tb + 1) * 512],
                                 start=(kc == 0), stop=(kc == KC - 1), reads=[w, x1T], writes=[psg])
                        psu = psH.next()
                        for kc in range(KC):
                            S.mm(psu[:], w[:, 1, kc, fc:256:2], x1T[:, kc, tb * 512:(tb + 1) * 512],
                                 start=(kc == 0), stop=(kc == KC - 1), reads=[w, x1T], writes=[psu])
                        sg = sgs.next()
                        S.op("act", lambda: nc.scalar.activation(sg[:], psg[:], AF.Silu), reads=[psg], writes=[sg])
                        t1 = t1s.next()
                        S.op("dve", lambda: nc.vector.tensor_tensor(out=t1[:], in0=psu[:], in1=sg[:], op=ALU.mult),
                             reads=[psu, sg], writes=[t1])
                        S.op("pool", lambda: nc.gpsimd.tensor_tensor(
                            out=a[:, j, fc, tb * 512:(tb + 1) * 512], in0=t1[:], in1=cbt[:, tb * 512:(tb + 1) * 512],
                            op=ALU.mult), reads=[t1, cbt], writes=[a])
            for ti in range(8):
                for db in range(4):
                    psy = psY.next()
                    n = 0
                    for j in range(2):
                        for fc in range(2):
                            S.mm(psy[:], a[:, j, fc, ti * 128:(ti + 1) * 128], wdt[:, j, fc, db * 512:(db + 1) * 512],
                                 start=(n == 0), stop=(n == 3), reads=[a, wdt], writes=[psy])
                            n += 1
                    yp = yparts[ti][db]
                    ysl = yacc[:, ti, db * 512:(db + 1) * 512]
                    if ep == 0:
                        S.copy(next(ev), ysl, psy[:], reads=[psy], writes=[yp])
                    else:
                        S.op("dve", lambda: nc.vector.tensor_tensor(out=ysl, in0=psy[:], in1=ysl, op=ALU.add),
                             reads=[psy, yp], writes=[yp])
      with S.phase():
        lng = S.sbp("lng2", [128, D], F32)
        lnb = S.sbp("lnb2", [128, D], F32)
        S.dma("sp", lng[:], I["ln2_g"][l].partition_broadcast(128), writes=[lng])
        S.dma("sp", lnb[:], I["ln2_b"][l].partition_broadcast(128), writes=[lnb])
        xts = Ring([S.sbp("x1r", [128, D], F32) for _ in range(3)])
        outs = Ring([S.sbp("x2t", [128, D], F32) for _ in range(3)])
        sts2 = Ring([S.sbp("st2", [128, 4, 6], F32) for _ in range(3)])
        mvs2 = Ring([S.sbp("mv2", [128, 8], F32) for _ in range(3)])
        ln_state = {}

        def ln2_s1(ti):
            i = blk * 8 + ti
            xt = xts.next()
            S.dma("sp", xt[:], R["x1"][i * 128:(i + 1) * 128, :], writes=[xt])
            ytok = S.tok("ytile")
            for db in range(4):
                S.op("dve", lambda: nc.vector.scalar_tensor_tensor(
                    out=yacc[:, ti, db * 512:(db + 1) * 512], in0=xt[:, db * 512:(db + 1) * 512], scalar=ALPHA,
                    in1=yacc[:, ti, db * 512:(db + 1) * 512], op0=ALU.mult, op1=ALU.add),
                    reads=[xt, yparts[ti][db]], writes=[yparts[ti][db], ytok])
            yv = Buf("yv", None)
            yv.last_write, yv.reads = ytok.last_write, []
            yv.__class__ = type("YV", (Buf,), {"__getitem__": lambda self, idx, ti=ti: yacc[(slice(None), ti) + tuple(idx[1:])] if isinstance(idx, tuple) else yacc[:, ti]})
            mv = mvs2.next()
            _layernorm_tile(C, yv, lng, lnb, xt, None, sts2.next(), mv, part=1)
            ln_state[ti] = (yv, xt, mv)

        def ln2_s2(ti):
            i = blk * 8 + ti
            yv, xt, mv = ln_state.pop(ti)
            ot = outs.next()
            _ln_part2(C, yv, lng, lnb, xt, ot, mv)
            S.dma("sp", x_out[i * 128:(i + 1) * 128, :], ot[:], reads=[ot])

        ln2_s1(0)
        for ti in range(8):
            if ti + 1 < 8:
                ln2_s1(ti + 1)
            ln2_s2(ti)
```
